# Optimizing a Trainium2 kernel written in Bass

```python
import jax, jax.numpy as jnp
from jax import lax
import numpy as np

D_MODEL = 2048
BATCH = 4
SEQ = 4096
DEPTH = 4
DEC_BATCH = 8
DEC_SEQ = 32
PAST_LEN = 2048

CHUNK = 64
D_A = D_MODEL
N_HEADS_A = 4
DH_A = D_A // N_HEADS_A
D_B = D_MODEL // 2
POOL_WINDOWS = (2, 4, 8, 16)
N_POOL_GROUPS = 4
POOL_GROUP = D_B // N_POOL_GROUPS
POOL_HIST = 15
IN_SIZES = (D_A, D_A, D_A, D_A, D_A, 2 * N_HEADS_A, D_B, D_B, D_MODEL, D_MODEL)
N_IN = 5 * D_A + 2 * N_HEADS_A + 2 * D_B + 2 * D_MODEL
EPS = 1e-6

kernel_name = 'hybrid_mlstm_pool_stream_step'


def rmsnorm(x, w):
    x32 = x.astype(jnp.float32)
    y = x32 * lax.rsqrt(jnp.mean(x32 * x32, axis=-1, keepdims=True) + EPS)
    return (y * w.astype(jnp.float32)).astype(x.dtype)


def head_layernorm(h, w):
    mu = jnp.mean(h, axis=-1, keepdims=True)
    d = h - mu
    var = jnp.mean(d * d, axis=-1, keepdims=True)
    y = d * lax.rsqrt(var + EPS)
    return y.reshape(h.shape[0], h.shape[1], D_A) * w.astype(jnp.float32)


def split_cols(p):
    outs = []
    start = 0
    for s in IN_SIZES:
        outs.append(p[..., start:start + s])
        start += s
    return outs


def mlstm_chunk(q, k, v, ig, lf, c0, n0, m0):
    L = q.shape[2]
    b = jnp.cumsum(lf, axis=-1)
    causal = jnp.tril(jnp.ones((L, L), dtype=bool))
    logw = jnp.where(causal, b[..., :, None] - b[..., None, :] + ig[..., None, :], -jnp.inf)
    inter = b + m0[..., None]
    m = jnp.maximum(inter, jnp.max(logw, axis=-1))
    w_intra = jnp.exp(logw - m[..., None])
    a_inter = jnp.exp(inter - m)
    s = jnp.einsum('bhtd,bhsd->bhts', q, k) * w_intra
    num = a_inter[..., None] * jnp.einsum('bhtd,bhde->bhte', q, c0) + jnp.einsum('bhts,bhse->bhte', s, v)
    den = a_inter * jnp.einsum('bhtd,bhd->bht', q, n0) + jnp.sum(s, axis=-1)
    h = num / jnp.maximum(jnp.abs(den), jnp.exp(-m))[..., None]
    m_end = m[..., -1]
    w_end = jnp.exp(b[..., -1:] - b + ig - m_end[..., None])
    decay = jnp.exp(b[..., -1] + m0 - m_end)
    c1 = decay[..., None, None] * c0 + jnp.einsum('bhs,bhsd,bhse->bhde', w_end, k, v)
    n1 = decay[..., None] * n0 + jnp.einsum('bhs,bhsd->bhd', w_end, k)
    return h, c1, n1, m_end


def mlstm_sequence(q, k, v, ig, lf, c0, n0, m0):
    B, H, L, DH = q.shape
    if L <= CHUNK:
        return mlstm_chunk(q, k, v, ig, lf, c0, n0, m0)
    nc = L // CHUNK
    qc = jnp.moveaxis(q.reshape(B, H, nc, CHUNK, DH), 2, 0)
    kc = jnp.moveaxis(k.reshape(B, H, nc, CHUNK, DH), 2, 0)
    vc = jnp.moveaxis(v.reshape(B, H, nc, CHUNK, DH), 2, 0)
    igc = jnp.moveaxis(ig.reshape(B, H, nc, CHUNK), 2, 0)
    lfc = jnp.moveaxis(lf.reshape(B, H, nc, CHUNK), 2, 0)

    def body(carry, xs):
        c, n, m = carry
        qq, kk, vv, ii, ff = xs
        h, c, n, m = mlstm_chunk(qq, kk, vv, ii, ff, c, n, m)
        return (c, n, m), h

    (c1, n1, m1), hs = lax.scan(body, (c0, n0, m0), (qc, kc, vc, igc, lfc))
    h = jnp.moveaxis(hs, 0, 2).reshape(B, H, L, DH)
    return h, c1, n1, m1


def pool_mixer(u, hist, pos0, w_mix, scale):
    B, L, _ = u.shape
    full = jnp.concatenate([hist.astype(jnp.float32), u.astype(jnp.float32)], axis=1)
    cs = jnp.concatenate([jnp.zeros((B, 1, D_B), jnp.float32), jnp.cumsum(full, axis=1)], axis=1)
    end = cs[:, POOL_HIST + 1:]
    pos = pos0 + jnp.arange(L) + 1
    outs = []
    for g, w in enumerate(POOL_WINDOWS):
        sl = slice(g * POOL_GROUP, (g + 1) * POOL_GROUP)
        start = cs[:, POOL_HIST + 1 - w:POOL_HIST + 1 - w + L, sl]
        cnt = jnp.minimum(w, pos).astype(jnp.float32)
        outs.append((end[..., sl] - start) / cnt[None, :, None])
    pooled = jnp.concatenate(outs, axis=-1)
    d = (pooled - u.astype(jnp.float32)).reshape(B, L, N_POOL_GROUPS, POOL_GROUP)
    mixed = jnp.einsum('blgc,gcd->blgd', d, w_mix.astype(jnp.float32)).reshape(B, L, D_B)
    return mixed * scale.astype(jnp.float32), full[:, -POOL_HIST:]


def mixer_layer(x, c, c0, n0, m0, hist, pos0, norm_w, w_ada, b_ada, w_in, b_if, head_norm_w,
                w_pool_mix, pool_scale, w_branch_a, w_branch_b, w_out):
    B, L, _ = x.shape
    mod = jnp.einsum('bd,de->be', jax.nn.silu(c), w_ada) + b_ada
    shift, scl, gate = jnp.split(mod, 3, axis=-1)
    h = rmsnorm(x, norm_w) * (1 + scl[:, None, :]) + shift[:, None, :]
    proj = jnp.einsum('bld,de->ble', h, w_in)
    q, k, v, o, z_a, ifg, u, z_b, g_a, g_b = split_cols(proj)

    def heads(t):
        return t.reshape(B, L, N_HEADS_A, DH_A).transpose(0, 2, 1, 3).astype(jnp.float32)

    ifg = ifg.astype(jnp.float32) + b_if.astype(jnp.float32)
    ig = ifg[..., :N_HEADS_A].transpose(0, 2, 1)
    lf = jax.nn.log_sigmoid(ifg[..., N_HEADS_A:]).transpose(0, 2, 1)
    hm, c1, n1, m1 = mlstm_sequence(heads(q), heads(k) * (DH_A ** -0.5), heads(v), ig, lf, c0, n0, m0)
    hm = jax.nn.sigmoid(o.astype(jnp.float32)).reshape(B, L, N_HEADS_A, DH_A) * hm.transpose(0, 2, 1, 3)
    a = (jax.nn.silu(z_a.astype(jnp.float32)) * head_layernorm(hm, head_norm_w)).astype(x.dtype)

    pb, new_hist = pool_mixer(u, hist, pos0, w_pool_mix, pool_scale)
    bb = (jax.nn.silu(z_b.astype(jnp.float32)) * pb).astype(x.dtype)

    merged = (jax.nn.sigmoid(g_a) * jnp.einsum('ble,ed->bld', a, w_branch_a)
              + jax.nn.sigmoid(g_b) * jnp.einsum('ble,ed->bld', bb, w_branch_b))
    y = x + gate[:, None, :] * jnp.einsum('bld,de->ble', merged, w_out)
    return y, c1, n1, m1, new_hist


def setup_inputs(seed: int = 0) -> dict:
    key = jax.random.key(seed)
    ks = jax.random.split(key, 24)
    f32 = jnp.float32
    nrm = lambda k, s: jax.random.normal(k, s, f32)
    b_if = jnp.concatenate([0.1 * nrm(ks[14], (DEPTH, N_HEADS_A)),
                            jnp.linspace(3.0, 6.0, N_HEADS_A)[None, :] + 0.1 * nrm(ks[15], (DEPTH, N_HEADS_A))], axis=-1)
    return {
        'x_prompt': nrm(ks[0], (BATCH, SEQ, D_MODEL)),
        'x_sample': nrm(ks[1], (DEC_BATCH, DEC_SEQ, D_MODEL)),
        'c_prompt': nrm(ks[2], (BATCH, D_MODEL)),
        'c_sample': nrm(ks[3], (DEC_BATCH, D_MODEL)),
        'state_C': 0.5 * DH_A ** -0.5 * nrm(ks[4], (DEPTH, DEC_BATCH, N_HEADS_A, DH_A, DH_A)),
        'state_n': 0.1 * nrm(ks[5], (DEPTH, DEC_BATCH, N_HEADS_A, DH_A)),
        'state_m': 0.5 * nrm(ks[6], (DEPTH, DEC_BATCH, N_HEADS_A)),
        'state_pool': nrm(ks[7], (DEPTH, DEC_BATCH, POOL_HIST, D_B)),
        'norm_w': 1.0 + 0.02 * nrm(ks[8], (DEPTH, D_MODEL)),
        'w_ada': 0.5 * D_MODEL ** -0.5 * nrm(ks[9], (DEPTH, D_MODEL, 3 * D_MODEL)),
        'b_ada': 0.02 * nrm(ks[10], (DEPTH, 3 * D_MODEL)),
        'w_in': D_MODEL ** -0.5 * nrm(ks[11], (DEPTH, D_MODEL, N_IN)),
        'b_if': b_if,
        'head_norm_w': 1.0 + 0.02 * nrm(ks[12], (DEPTH, D_A)),
        'w_pool_mix': POOL_GROUP ** -0.5 * nrm(ks[13], (DEPTH, N_POOL_GROUPS, POOL_GROUP, POOL_GROUP)),
        'pool_scale': 1.0 + 0.1 * nrm(ks[16], (DEPTH, D_B)),
        'w_branch_a': D_A ** -0.5 * nrm(ks[17], (DEPTH, D_A, D_MODEL)),
        'w_branch_b': D_B ** -0.5 * nrm(ks[18], (DEPTH, D_B, D_MODEL)),
        'w_out': D_MODEL ** -0.5 * nrm(ks[19], (DEPTH, D_MODEL, D_MODEL)),
        'final_norm_w': 1.0 + 0.02 * nrm(ks[20], (D_MODEL,)),
    }


def reference(x_prompt, x_sample, c_prompt, c_sample, state_C, state_n, state_m, state_pool,
              norm_w, w_ada, b_ada, w_in, b_if, head_norm_w, w_pool_mix, pool_scale,
              w_branch_a, w_branch_b, w_out, final_norm_w):
    f32 = jnp.float32
    bp = x_prompt.shape[0]
    yp = x_prompt
    ys = x_sample
    cp_l, np_l, mp_l, hp_l = [], [], [], []
    cs_l, ns_l, ms_l, hs_l = [], [], [], []
    for l in range(DEPTH):
        params = (norm_w[l], w_ada[l], b_ada[l], w_in[l], b_if[l], head_norm_w[l],
                  w_pool_mix[l], pool_scale[l], w_branch_a[l], w_branch_b[l], w_out[l])
        yp, c1, n1, m1, h1 = mixer_layer(
            yp, c_prompt,
            jnp.zeros((bp, N_HEADS_A, DH_A, DH_A), f32), jnp.zeros((bp, N_HEADS_A, DH_A), f32),
            jnp.zeros((bp, N_HEADS_A), f32), jnp.zeros((bp, POOL_HIST, D_B), x_prompt.dtype), 0, *params)
        cp_l.append(c1); np_l.append(n1); mp_l.append(m1); hp_l.append(h1)
        ys, c2, n2, m2, h2 = mixer_layer(
            ys, c_sample, state_C[l].astype(f32), state_n[l].astype(f32), state_m[l].astype(f32),
            state_pool[l], PAST_LEN, *params)
        cs_l.append(c2); ns_l.append(n2); ms_l.append(m2); hs_l.append(h2)
    y_prompt = rmsnorm(yp, final_norm_w)
    y_sample = rmsnorm(ys, final_norm_w)
    pdt = x_prompt.dtype
    new_C_prompt = jnp.stack(cp_l).astype(pdt)
    new_n_prompt = jnp.stack(np_l).astype(pdt)
    new_m_prompt = jnp.stack(mp_l).astype(pdt)
    new_pool_prompt = jnp.stack(hp_l).astype(pdt)
    new_C_sample = jnp.stack(cs_l).astype(state_C.dtype)
    new_n_sample = jnp.stack(ns_l).astype(state_n.dtype)
    new_m_sample = jnp.stack(ms_l).astype(state_m.dtype)
    new_pool_sample = jnp.stack(hs_l).astype(state_pool.dtype)
    return (y_prompt, y_sample, new_C_prompt, new_n_prompt, new_m_prompt, new_pool_prompt,
            new_C_sample, new_n_sample, new_m_sample, new_pool_sample)
```

```python
import numpy as np
import concourse.bass as bass
import concourse.mybir as mybir
from concourse.bass_utils import run_bass_kernel_spmd

F32 = mybir.dt.float32
BF16 = mybir.dt.bfloat16
AF = mybir.ActivationFunctionType
ALU = mybir.AluOpType

D = 2048
KT = 16
NH = 4
DH = 512
DB = 1024
HIST = 15
NIN = 16392
OFF_Q, OFF_K, OFF_V, OFF_O, OFF_ZA, OFF_IF, OFF_U, OFF_ZB, OFF_GA, OFF_GB = (
    0, 2048, 4096, 6144, 8192, 10240, 10248, 11272, 12296, 14344)
EPS = 1e-6
WCOLS = 512
NSLOT = 4
ENGS = ['pe', 'act', 'dve', 'pool', 'sp']


class Slot:
    __slots__ = ('name', 'excl', 'last_w', 'readers', 'alias')

    def __init__(self, name, excl=False):
        self.name = name
        self.excl = excl
        self.last_w = None
        self.readers = {}
        self.alias = [self]


def alias_group(slots):
    for s in slots:
        s.alias = list(slots)


class Op:
    __slots__ = ('eng', 'fn', 'idx', 'is_dma', 'dkey', 'dval', 'need_inc', 'semval', 'waits')


class Prog:
    def __init__(self, nc):
        self.nc = nc
        self.ops = {e: [] for e in ENGS}
        self.eng_sem = {e: nc.alloc_semaphore(name="sem_" + e) for e in ENGS}
        self.dma_sem = {}
        self.dma_cnt = {}
        self.seen = {e: {} for e in ENGS}

    def add(self, eng, fn, reads=(), writes=(), dma_key=None):
        op = Op()
        op.eng = eng
        op.fn = fn
        op.idx = len(self.ops[eng])
        op.is_dma = dma_key is not None
        op.dkey = dma_key
        op.need_inc = False
        op.semval = 0
        op.dval = 0
        if op.is_dma:
            if dma_key not in self.dma_sem:
                self.dma_sem[dma_key] = self.nc.alloc_semaphore(name="dsem_%d" % len(self.dma_sem))
                self.dma_cnt[dma_key] = 0
            self.dma_cnt[dma_key] += 16
            op.dval = self.dma_cnt[dma_key]
        deps = []
        for s in reads:
            for a in s.alias:
                if a.last_w is not None:
                    deps.append(a.last_w)
                if a.excl:
                    for r in a.readers.values():
                        if r.eng != eng:
                            deps.append(r)
        for s in writes:
            for a in s.alias:
                if a.last_w is not None:
                    deps.append(a.last_w)
                deps.extend(a.readers.values())
        waits = {}
        seen = self.seen[eng]
        for d in deps:
            if d is op:
                continue
            if d.is_dma:
                key = ('d', d.dkey)
                val = d.dval
            else:
                if d.eng == eng and not op.is_dma:
                    if eng == 'pe':
                        continue
                    if op.idx - d.idx > 3:
                        continue
                key = ('e', d.eng)
                val = d.idx
            if seen.get(key, -1) >= val:
                continue
            if key not in waits or waits[key].__getattribute__('dval' if d.is_dma else 'idx') < val:
                waits[key] = d
        for key, d in waits.items():
            seen[key] = d.dval if d.is_dma else d.idx
            d.need_inc = True
        op.waits = list(waits.values())
        rkey = ('d', dma_key) if op.is_dma else ('e', eng)
        for s in reads:
            s.readers[rkey] = op
        for s in writes:
            s.last_w = op
            s.readers = {}
        self.ops[eng].append(op)
        return op

    def dma(self, eng, out, in_, key, reads=(), writes=()):
        return self.add(eng, lambda e: e.dma_start(out=out, in_=in_), reads, writes, dma_key=key)

    def emit(self):
        nc = self.nc
        for e in ENGS:
            c = 0
            for op in self.ops[e]:
                if (not op.is_dma) and op.need_inc:
                    c += 1
                    op.semval = c
        final_waits = [(self.dma_sem[k], v) for k, v in self.dma_cnt.items()]

        def run(e, eng):
            for op in self.ops[e]:
                for d in op.waits:
                    if d.is_dma:
                        eng.wait_ge(self.dma_sem[d.dkey], d.dval)
                    else:
                        eng.wait_ge(self.eng_sem[d.eng], d.semval)
                ins = op.fn(eng)
                if op.is_dma:
                    ins.then_inc(self.dma_sem[op.dkey], 16)
                elif op.need_inc:
                    ins.then_inc(self.eng_sem[e], 1)
            if e == 'sp':
                for sem, v in final_waits:
                    eng.wait_ge(sem, v)

        with nc.allow_non_contiguous_dma(reason="tiny strided state/bias transfers"), nc.Block() as block:
            @block.tensor
            def _(eng):
                run('pe', eng)

            @block.scalar
            def _(eng):
                run('act', eng)

            @block.vector
            def _(eng):
                run('dve', eng)

            @block.gpsimd
            def _(eng):
                run('pool', eng)

            @block.sync
            def _(eng):
                run('sp', eng)


def build_program(T, NS, SL, L, MT=512):
    nc = bass.Bass("TRN2", target_bir_lowering=False)
    P = Prog(nc)
    NSEQ = 1 + NS
    NR = 6 * L + 1 + NSEQ + L * NS
    assert NR <= 128 and T % MT == 0 and MT % 128 == 0

    def r_norm(l): return l
    def r_ada(l, w): return L + 3 * l + w
    def r_hn(l): return 4 * L + l
    def r_ps(l): return 5 * L + l
    r_fin = 6 * L
    def r_c(q): return 6 * L + 1 + q
    def r_n(l, s): return 6 * L + 1 + NSEQ + l * NS + s

    def din(name, shape):
        return nc.dram_tensor(name, list(shape), F32, kind="ExternalInput").ap()

    def dout(name, shape):
        return nc.dram_tensor(name, list(shape), F32, kind="ExternalOutput").ap()

    xp = din("xp", [T, D])
    xs = din("xs", [NS, SL, D])
    rows_d = din("rows", [NR, D])
    bif_d = din("bif", [L, 8])
    stm_d = din("st_m", [L, NS, NH])
    stC_d = din("st_C", [L, NS, NH, DH, DH])
    stP_d = din("st_pool", [L, NS, HIST, DB])
    wada_d = din("w_ada", [L, D, 3 * D])
    win_d = din("w_in", [L, D, NIN])
    wpm_d = din("w_pm", [L, 4, 256, 256])
    wa_d = din("w_a", [L, D, D])
    wb_d = din("w_b", [L, DB, D])
    wo_d = din("w_o", [L, D, D])
    cf_d = din("cf", [128, 320])
    sel_d = din("sel", [4, 4 * 128])

    yp_d = dout("yp", [T, D])
    ys_d = dout("ys", [NS, SL, D])
    Cp_d = dout("Cp", [L, NH, DH, DH])
    np_d = dout("np", [L, NH * 4, 128])
    mp_d = dout("mp", [L, NH])
    pp_d = dout("pp", [L, HIST, DB])
    Cs_d = dout("Cs", [L, NS, NH, DH, DH])
    ns_d = dout("ns", [L, NS, NH * 4, 128])
    ms_d = dout("ms", [L, NS, NH])
    ps_d = dout("ps", [L, NS, HIST, DB])

    xres_p = nc.dram_tensor("xres_p", [KT, 128, T], F32, kind="Internal").ap()
    xres_s = nc.dram_tensor("xres_s", [NS, KT, 128, SL], F32, kind="Internal").ap()

    def sb(name, shape, dt):
        return nc.alloc_sbuf_tensor("sb_" + name, list(shape), dt)

    hT = sb("hT", [128, KT, MT], BF16); s_hT = Slot("hT")
    aT = sb("aT", [128, KT, MT], BF16); s_aT = Slot("aT")
    bbT = sb("bbT", [128, 8, MT], BF16); s_bbT = Slot("bbT")
    Cst = sb("Cst", [128, NH, 4, DH], F32)
    s_Cst = [[Slot("Cst%d_%d" % (h, dt)) for dt in range(4)] for h in range(NH)]
    Cbf = sb("Cbf", [128, 4, DH], BF16)
    s_Cbf = [Slot("Cbf%d" % dt) for dt in range(4)]
    nst = sb("nst", [128, NH, 4], F32); s_nst = Slot("nst")
    nbf = sb("nbf", [128, NH, 4], BF16); s_nbf = Slot("nbf")
    ring = [sb("ring%d" % i, [128, KT, WCOLS], BF16) for i in range(NSLOT)]
    wg = sb("wg", [128, KT, 8], BF16); s_wg = Slot("wg")
    wpm = sb("wpm", [128, 4, 2, 256], BF16); s_wpm = Slot("wpm")
    cf = sb("cf", [128, 320], F32); s_cf = Slot("cf")
    ident_f = cf[:, 0:128]
    maskneg = cf[:, 128:256]
    invcnt = cf[:, 256:320]
    sel = sb("sel", [4, 4 * 128], F32); s_sel = Slot("sel")
    ident_bf = sb("ident_bf", [128, 128], BF16); s_identbf = Slot("identbf")
    ones_bf = sb("ones_bf", [128, 128], BF16); s_onesbf = Slot("onesbf")
    ones4 = sb("ones4", [4, 128], F32); s_ones4 = Slot("ones4")
    epsc = sb("epsc", [128, 1], F32); s_epsc = Slot("epsc")
    colsT = sb("colsT", [128, KT, NR], F32); s_colsT = Slot("colsT")
    csT = sb("csT", [128, KT, NSEQ], BF16); s_csT = Slot("csT")
    bif = sb("bif", [4, L, 2], F32); s_bif = Slot("bif")
    nbf_f = sb("nbf_f", [4, L], F32); s_nbff = Slot("nbff")
    mst = sb("mst", [4, L * NS], F32); s_mst = Slot("mst")
    mcar = sb("mcar", [4, 1], F32); s_mcar = Slot("mcar")
    modT = sb("modT", [128, 48, NSEQ], F32); s_modT = Slot("modT")
    AmodT = sb("AmodT", [128, KT, NSEQ], F32); s_AmodT = Slot("AmodT")
    g_ig = sb("g_ig", [4, MT], F32); s_gig = Slot("g_ig")
    g_lf = sb("g_lf", [4, MT], F32); s_glf = Slot("g_lf")
    g_B = sb("g_B", [4, MT], F32); s_gB = Slot("g_B")
    g_m = sb("g_m", [4, MT], F32); s_gm = Slot("g_m")
    g_g, g_c, s_gg, s_gc = g_ig, g_B, s_gig, s_gB
    NCH = MT // 128
    cend = sb("cend", [4, NCH + 1], F32); s_cend = Slot("cend")
    cediag = sb("cediag", [4, NH, NCH + 1], F32); s_cediag = Slot("cediag")
    CE = sb("CE", [128, NH, NCH + 1], F32); s_CE = Slot("CE")
    cols = sb("cols", [128, NCH, 3, NH], F32); s_cols = Slot("cols")
    acol = sb("acol", [128, NCH, NH], F32); s_acol = Slot("acol")
    emcol = sb("emcol", [128, NCH, NH], F32); s_emcol = Slot("emcol")
    wecol = sb("wecol", [128, NCH, NH], F32); s_wecol = Slot("wecol")
    dccol = sb("dccol", [128, NCH, NH], F32); s_dccol = Slot("dccol")
    tmpc = sb("tmpc", [128, NCH, NH], F32); s_tmpc = Slot("tmpc")
    small = sb("small", [128, 2, 16], F32)
    s_small = [Slot("small0"), Slot("small1")]
    bnst = sb("bnst", [128, 2, 8], F32)
    s_bnst = [Slot("bnst0"), Slot("bnst1")]
    rstd_bc = sb("rstd_bc", [128, MT], F32); s_rstd = Slot("rstd")

    UN = 18432
    U = sb("U", [128, UN], BF16)

    class Reg:
        def __init__(self):
            self.off = 0

        def take(self, name, nelem, dt):
            n16 = nelem * (2 if dt == F32 else 1)
            o = self.off
            self.off += n16
            assert self.off <= UN, (name, self.off)
            ap = U[:, o:o + n16]
            if dt == F32:
                ap = ap.bitcast(F32)
            return ap

    s_U = Slot("U_phase")

    rb = Reg()
    qT = rb.take("qT", 4 * MT, BF16).rearrange("p (a b) -> p a b", a=4)
    kT = rb.take("kT", 4 * MT, BF16).rearrange("p (a b) -> p a b", a=4)
    vtk = rb.take("v", NCH * DH, BF16).rearrange("p (a b) -> p a b", a=NCH)
    sgo = rb.take("sgo", NCH * DH, BF16).rearrange("p (a b) -> p a b", a=NCH)
    sza = rb.take("sza", NCH * DH, BF16).rearrange("p (a b) -> p a b", a=NCH)
    A2 = [rb.take("A2_%d" % i, DH, F32) for i in range(2)]
    hm = [rb.take("hm_%d" % i, DH, F32) for i in range(2)]
    atok = [rb.take("atok_%d" % i, DH, BF16) for i in range(2)]
    kw = [rb.take("kw_%d" % i, DH, BF16) for i in range(2)]
    DTm = [rb.take("DT_%d" % i, 128, F32) for i in range(2)]
    PTm = [rb.take("PT_%d" % i, 128, BF16) for i in range(2)]
    sB = {n: Slot("B_" + n) for n in ["qT", "kT", "v", "sgo", "sza"]}
    sB2 = {n: [Slot("B_%s%d" % (n, i)) for i in range(2)] for n in ["A2", "hm", "atok", "kw", "DT", "PT"]}
    rc = Reg()
    UW = HIST + MT
    uT2 = [rc.take("uT%d" % i, UW, F32) for i in range(2)]
    zbT = rc.take("zbT", 8 * MT, BF16).rearrange("p (a b) -> p a b", a=8)
    dT = rc.take("dT", 8 * MT, BF16).rearrange("p (a b) -> p a b", a=8)
    ptmp = [rc.take("ptmp%d" % i, UW, F32) for i in range(2)]
    sC = {n: Slot("C_" + n) for n in ["uT0", "uT1", "zbT", "dT", "ptmp0", "ptmp1"]}
    hist_in = zbT.rearrange("p a b -> p (a b)")[:, 0:2 * DB].bitcast(F32)
    hist_out = dT.rearrange("p a b -> p (a b)")[:, 0:2 * DB].bitcast(F32)
    rd = Reg()
    mgT = rd.take("mgT", KT * MT, BF16).rearrange("p (a b) -> p a b", a=KT)
    sg = [rd.take("sg%d" % i, MT, F32) for i in range(4)]
    t12 = [rd.take("t12_%d" % i, MT, F32) for i in range(2)]
    xin = [rd.take("xin%d" % i, MT, F32) for i in range(2)]
    xout = [rd.take("xout%d" % i, MT, F32) for i in range(2)]
    sD = {"mgT": Slot("D_mgT")}
    for i in range(4):
        sD["sg%d" % i] = Slot("D_sg%d" % i)
    for i in range(2):
        sD["t12_%d" % i] = Slot("D_t12_%d" % i)
        sD["xin%d" % i] = Slot("D_xin%d" % i)
        sD["xout%d" % i] = Slot("D_xout%d" % i)
    ra = Reg()
    xa = [ra.take("xa%d" % i, MT, F32) for i in range(2)]
    sqb = [ra.take("sqb%d" % i, MT, BF16) for i in range(2)]
    ta = [ra.take("ta%d" % i, MT, F32) for i in range(2)]
    sA = {}
    for i in range(2):
        sA["xa%d" % i] = Slot("A_xa%d" % i)
        sA["sqb%d" % i] = Slot("A_sqb%d" % i)
        sA["ta%d" % i] = Slot("A_ta%d" % i)
    ri = Reg()
    rows_sb = ri.take("rows", D, F32)
    xtok = ri.take("xtok", D, F32)
    xo4 = [ri.take("xo4_%d" % i, 512, F32).rearrange("p (a b) -> p a b", a=4) for i in range(2)]
    sI = {"rows": Slot("I_rows"), "xtok": Slot("I_xtok"), "xo4_0": Slot("I_xo4_0"), "xo4_1": Slot("I_xo4_1")}
    rf = Reg()
    fxa = [rf.take("fxa%d" % i, MT, F32) for i in range(2)]
    fsq = [rf.take("fsq%d" % i, MT, BF16) for i in range(2)]
    fta = [rf.take("fta%d" % i, MT, F32) for i in range(2)]
    ytok = [rf.take("ytok%d" % i, D, F32) for i in range(2)]
    sF = {}
    for i in range(2):
        for n in ["fxa", "fsq", "fta", "ytok"]:
            sF["%s%d" % (n, i)] = Slot("F_%s%d" % (n, i))
    allU = (list(sB.values()) + [x for v in sB2.values() for x in v] + list(sC.values()) +
            list(sD.values()) + list(sA.values()) + list(sI.values()) + list(sF.values()))
    phases = [list(sB.values()) + [x for v in sB2.values() for x in v], list(sC.values()),
              list(sD.values()), list(sA.values()), list(sI.values()), list(sF.values())]
    for pi, ph in enumerate(phases):
        others = [s for pj, q in enumerate(phases) if pj != pi for s in q]
        for s in ph:
            s.alias = [s] + others

    banks = [nc.alloc_psum_tensor("psb%d" % i, [128, 512], F32) for i in range(8)]
    s_bank = [Slot("bank%d" % i, excl=True) for i in range(8)]
    prj_rot = [0]

    def next_prj_bank():
        b = prj_rot[0]
        prj_rot[0] = (b + 1) % 2
        return b

    def mm(out, lhsT, rhs, start, stop, reads, writes):
        P.add('pe', lambda e: e.matmul(out, lhsT, rhs, start=start, stop=stop), reads, writes)

    def tr(out, in_, ident, reads, writes):
        P.add('pe', lambda e: e.transpose(out, in_, ident), reads, writes)

    def act(out, in_, func, reads, writes, bias=None, scale=None):
        kw_ = {}
        if bias is not None:
            kw_['bias'] = bias
        if scale is not None:
            kw_['scale'] = scale
        P.add('act', lambda e: e.activation(out=out, in_=in_, func=func, **kw_), reads, writes)

    def tt(out, in0, in1, op, reads, writes, eng='dve'):
        P.add(eng, lambda e: e.tensor_tensor(out=out, in0=in0, in1=in1, op=op), reads, writes)

    def ts(out, in0, s1, s2, op0, op1, reads, writes, eng='dve'):
        if op1 is None:
            P.add(eng, lambda e: e.tensor_scalar(out=out, in0=in0, scalar1=s1, scalar2=None, op0=op0), reads, writes)
        else:
            P.add(eng, lambda e: e.tensor_scalar(out=out, in0=in0, scalar1=s1, scalar2=s2, op0=op0, op1=op1),
                  reads, writes)

    def stt(out, in0, scalar, in1, op0, op1, reads, writes):
        P.add('dve', lambda e: e.scalar_tensor_tensor(out=out, in0=in0, scalar=scalar, in1=in1, op0=op0, op1=op1),
              reads, writes)

    def cp(eng, out, in_, reads, writes):
        if eng == 'act':
            P.add('act', lambda e: e.activation(out=out, in_=in_, func=AF.Identity), reads, writes)
        else:
            P.add(eng, lambda e: e.tensor_copy(out=out, in_=in_), reads, writes)

    def memset(eng, ap, val, writes):
        P.add(eng, lambda e: e.memset(ap, val), (), writes)

    ring_ctr = [0]
    wcache_idx = {}
    NCACHE = 64
    wcache = nc.dram_tensor("wcache", [NCACHE, 128, KT * WCOLS], BF16, kind="Internal").ap()
    s_wcache = [Slot("wcache%d" % i) for i in range(NCACHE)]
    s_half = [Slot("ringh%d" % i) for i in range(2 * NSLOT)]

    def load_w(src_rows_ap, nk, ncols, blk_id=None):
        if ncols == WCOLS:
            if ring_ctr[0] % 2:
                ring_ctr[0] += 1
            h0 = ring_ctr[0] % (2 * NSLOT)
            ring_ctr[0] += 2
            hs = [s_half[h0], s_half[h0 + 1]]
            dst = ring[h0 // 2][:, 0:nk, :]
        else:
            assert ncols == WCOLS // 2
            h0 = ring_ctr[0] % (2 * NSLOT)
            ring_ctr[0] += 1
            hs = [s_half[h0]]
            dst = ring[h0 // 2][:, 0:nk, (h0 % 2) * ncols:(h0 % 2 + 1) * ncols]
        key = "ring%d" % h0
        if blk_id is not None and blk_id in wcache_idx:
            ci = wcache_idx[blk_id]
            P.dma('sp', dst, wcache[ci, :, 0:nk * ncols].rearrange("p (k c) -> p k c", k=nk), key,
                  [s_wcache[ci]], hs)
        else:
            src = src_rows_ap.rearrange("(k p) c -> p k c", p=128)
            P.dma('pool', dst, src, key, (), hs)
            if blk_id is not None:
                ci = len(wcache_idx)
                assert ci < NCACHE
                wcache_idx[blk_id] = ci
                P.dma('pool', wcache[ci, :, 0:nk * ncols].rearrange("p (k c) -> p k c", k=nk), dst, "wst%d" % h0,
                      hs, [s_wcache[ci]])
        return hs, dst

    P.dma('sp', cf[:], cf_d[:, :], "cf", (), [s_cf])
    P.dma('sp', sel[:], sel_d[:, :], "sel", (), [s_sel])
    P.dma('sp', rows_sb[0:NR, :], rows_d[:, :], "rows", (), [sI["rows"]])
    P.dma('sp', bif[:], bif_d.rearrange("l (w k) -> k l w", w=2), "bif", (), [s_bif])
    P.dma('sp', mst[:], stm_d.rearrange("l s k -> k (l s)"), "mst", (), [s_mst])
    memset('dve', ones_bf[:], 1.0, [s_onesbf])
    memset('dve', ones4[:], 1.0, [s_ones4])
    memset('dve', epsc[:], EPS, [s_epsc])
    cp('dve', ident_bf[:], ident_f, [s_cf], [s_identbf])
    ts(nbf_f[:], bif[:, :, 1], -1.0, None, ALU.mult, None, [s_bif], [s_nbff])
    for kt in range(KT):
        b = next_prj_bank()
        tr(banks[b][:, 0:NR], rows_sb[0:NR, kt * 128:(kt + 1) * 128], ident_f[0:NR, 0:NR],
           [sI["rows"], s_cf], [s_bank[b]])
        cp('act', colsT[:, kt, :], banks[b][:, 0:NR], [s_bank[b]], [s_colsT])
    act(csT[:], colsT[:, :, r_c(0):r_c(0) + NSEQ], AF.Silu, [s_colsT], [s_csT])

    def prepass(src_tok_ap, ntok, dst_fn, key):
        P.dma('sp', xtok[0:ntok, :], src_tok_ap, "xtok", (), [sI["xtok"]])
        for g in range(4):
            b = next_prj_bank()
            for i in range(4):
                kt = g * 4 + i
                tr(banks[b][:, i * 128:i * 128 + ntok], xtok[0:ntok, kt * 128:(kt + 1) * 128],
                   ident_f[0:ntok, 0:ntok], [sI["xtok"], s_cf], [s_bank[b]])
            o = g % 2
            cp('act' if g % 2 == 0 else 'dve', xo4[o][:, :, 0:ntok],
               banks[b][:, :].rearrange("p (a b) -> p a b", a=4)[:, :, 0:ntok],
               [s_bank[b]], [sI["xo4_%d" % o]])
            P.dma('sp', dst_fn(g * 4), xo4[o][:, :, 0:ntok], "xo4_%d" % o, [sI["xo4_%d" % o]], ())

    for tb in range(T // 128):
        prepass(xp[tb * 128:(tb + 1) * 128, :], 128,
                lambda k0, tb=tb: xres_p[k0:k0 + 4, :, tb * 128:(tb + 1) * 128].rearrange("k p t -> p k t"), "pp")
    for s in range(NS):
        prepass(xs[s, :, :], SL,
                lambda k0, s=s: xres_s[s, k0:k0 + 4, :, :].rearrange("k p t -> p k t"), "ps")

    class Tile:
        pass

    tiles = []
    for j in range(T // MT):
        t = Tile()
        t.seq = 0; t.M = MT; t.Lc = 128; t.nch = MT // 128; t.t0 = j * MT
        t.first = (j == 0); t.last = (j == T // MT - 1); t.sample = None
        t.xres = lambda kt, t=t: xres_p[kt, :, t.t0:t.t0 + t.M]
        tiles.append(t)
    for s in range(NS):
        t = Tile()
        t.seq = 1 + s; t.M = SL; t.Lc = SL; t.nch = 1; t.t0 = 0
        t.first = True; t.last = True; t.sample = s
        t.xres = lambda kt, s=s: xres_s[s, kt, :, :]
        tiles.append(t)

    s_xres = {}

    def xres_slot(seq):
        if seq not in s_xres:
            s_xres[seq] = [Slot("xres%d_0" % seq), Slot("xres%d_1" % seq)]
        return s_xres[seq]

    pre_store_ops = [op for op in P.ops['sp'] if op.is_dma and op.dkey in ("xo4_0", "xo4_1")]
    last_pre = {}
    for op in pre_store_ops:
        last_pre[op.dkey] = op

    class _Multi:
        pass

    pre_slots = []
    for k, op in last_pre.items():
        sl = Slot("pre_" + k)
        sl.last_w = op
        pre_slots.append(sl)

    def rms_stats(tile, load_x, xbuf, xslots, sqbuf, sqslots):
        M = tile.M
        b = next_prj_bank()
        for kt in range(KT):
            i = kt % 2
            load_x(kt, xbuf[i], xslots[i])
            act(sqbuf[i][:, 0:M], xbuf[i][:, 0:M], AF.Square, [xslots[i]], [sqslots[i]])
            mm(banks[b][:, 0:M], ones_bf[:, :], sqbuf[i][:, 0:M], kt == 0, kt == KT - 1,
               [s_onesbf, sqslots[i]], [s_bank[b]])
        act(rstd_bc[:, 0:M], banks[b][:, 0:M], AF.Sqrt, [s_bank[b], s_epsc], [s_rstd],
            bias=epsc[:, 0:1], scale=1.0 / D)
        P.add('dve', lambda e: e.reciprocal(out=rstd_bc[:, 0:M], in_=rstd_bc[:, 0:M]), [s_rstd], [s_rstd])

    def layer(l):
        wcache_idx.clear()
        mb = 2
        for blk in range(3 * D // WCOLS):
            si, w = load_w(wada_d[l, :, blk * WCOLS:(blk + 1) * WCOLS], KT, WCOLS)
            for ct in range(WCOLS // 128):
                e = blk * (WCOLS // 128) + ct
                for kt in range(KT):
                    mm(banks[mb][:, e * NSEQ:(e + 1) * NSEQ], w[:, kt, ct * 128:(ct + 1) * 128], csT[:, kt, :],
                       kt == 0, kt == KT - 1, si + [s_csT], [s_bank[mb]])
        for wi in range(3):
            for q in range(NSEQ):
                src = banks[mb][:, wi * KT * NSEQ:(wi + 1) * KT * NSEQ].rearrange("p (k q) -> p k q", q=NSEQ)[:, :, q]
                tt(modT[:, wi * KT:(wi + 1) * KT, q], src, colsT[:, :, r_ada(l, wi)], ALU.add,
                   [s_bank[mb], s_colsT], [s_modT])
        for q in range(NSEQ):
            stt(AmodT[:, :, q], modT[:, KT:2 * KT, q], 1.0, colsT[:, :, r_norm(l)], ALU.add, ALU.mult,
                [s_modT, s_colsT], [s_AmodT])
        P.dma('pool', wpm[:], wpm_d[l].rearrange("g (k p) c -> p g k c", p=128), "wpm", (), [s_wpm])
        P.dma('pool', wg[:], win_d[l, :, OFF_IF:OFF_IF + 8].rearrange("(k p) c -> p k c", p=128), "wg", (), [s_wg])

        for tile in tiles:
            run_tile(l, tile)

    def run_tile(l, tile):
        M, Lc, nch, q = tile.M, tile.Lc, tile.nch, tile.seq
        xsl = xres_slot(q)

        def load_x(kt, buf, slot):
            P.dma('sp', buf[:, 0:M], tile.xres(kt), "ld_" + slot.name, xsl + pre_slots, [slot])

        rms_stats(tile, load_x, xa, [sA["xa0"], sA["xa1"]], sqb, [sA["sqb0"], sA["sqb1"]])
        for kt in range(KT):
            i = kt % 2
            load_x(kt, xa[i], sA["xa%d" % i])
            tt(ta[i][:, 0:M], xa[i][:, 0:M], rstd_bc[:, 0:M], ALU.mult, [sA["xa%d" % i], s_rstd], [sA["ta%d" % i]])
            act(hT[:, kt, 0:M], ta[i][:, 0:M], AF.Identity, [sA["ta%d" % i], s_AmodT, s_modT], [s_hT],
                bias=modT[:, kt, q:q + 1], scale=AmodT[:, kt, q:q + 1])

        stage_C(l, tile)
        stage_B(l, tile)
        stage_D(l, tile)

    def stage_C(l, tile):
        M, Lc, nch, q = tile.M, tile.Lc, tile.nch, tile.seq
        W = HIST + M
        if tile.first:
            if tile.sample is None:
                memset('dve', ucarry[:], 0.0, [s_ucarry])
            else:
                s = tile.sample
                P.dma('sp', hist_in[0:HIST, :], stP_d[l, s, :, :], "hist_in", (), [sC["zbT"]])
                for ct in range(8):
                    b = next_prj_bank()
                    tr(banks[b][:, 0:HIST], hist_in[0:HIST, ct * 128:(ct + 1) * 128], ident_f[0:HIST, 0:HIST],
                       [sC["zbT"], s_cf], [s_bank[b]])
                    cp('act', ucarry[:, ct, :], banks[b][:, 0:HIST], [s_bank[b]], [s_ucarry])
        for blk in range(DB // WCOLS):
            si, w = load_w(win_d[l, :, OFF_ZB + blk * WCOLS:OFF_ZB + (blk + 1) * WCOLS], KT, WCOLS, ('zb', blk))
            for ct in range(WCOLS // 128):
                c = blk * (WCOLS // 128) + ct
                b = next_prj_bank()
                for kt in range(KT):
                    mm(banks[b][:, 0:M], w[:, kt, ct * 128:(ct + 1) * 128], hT[:, kt, 0:M], kt == 0, kt == KT - 1,
                       si + [s_hT], [s_bank[b]])
                act(zbT[:, c, 0:M], banks[b][:, 0:M], AF.Silu, [s_bank[b]], [sC["zbT"]])
        for blk in range(DB // WCOLS):
            si, w = load_w(win_d[l, :, OFF_U + blk * WCOLS:OFF_U + (blk + 1) * WCOLS], KT, WCOLS, ('u', blk))
            for ct in range(WCOLS // 128):
                c = blk * (WCOLS // 128) + ct
                g = c // 2
                u = uT2[c % 2]
                us = sC["uT%d" % (c % 2)]
                b = next_prj_bank()
                for kt in range(KT):
                    mm(banks[b][:, 0:M], w[:, kt, ct * 128:(ct + 1) * 128], hT[:, kt, 0:M], kt == 0, kt == KT - 1,
                       si + [s_hT], [s_bank[b]])
                cp('dve', u[:, 0:HIST], ucarry[:, c, :], [s_ucarry], [us])
                cp('act', u[:, HIST:W], banks[b][:, 0:M], [s_bank[b]], [us])
                cp('dve', ucarry[:, c, :], u[:, M:W], [us], [s_ucarry])
                cur, curslot = u, us
                sh = 1
                for step in range(g + 1):
                    o = ptmp[step % 2]
                    oslot = sC["ptmp%d" % (step % 2)]
                    lo = 2 * sh - 1
                    tt(o[:, lo:W], cur[:, lo:W], cur[:, lo - sh:W - sh], ALU.add, [curslot], [oslot])
                    cur, curslot = o, oslot
                    sh *= 2
                wlen = 2 ** (g + 1)
                stt(dT[:, c, 0:M], cur[:, HIST:W], 1.0 / wlen, u[:, HIST:W], ALU.mult, ALU.subtract,
                    [curslot, us], [sC["dT"]])
                if tile.first and tile.sample is None:
                    n = min(HIST, M)
                    o2 = ptmp[(g + 1) % 2]
                    o2slot = sC["ptmp%d" % ((g + 1) % 2)]
                    tt(o2[:, 0:n], cur[:, HIST:HIST + n], invcnt[:, g * 16:g * 16 + n], ALU.mult, [curslot, s_cf],
                       [o2slot])
                    tt(dT[:, c, 0:n], o2[:, 0:n], u[:, HIST:HIST + n], ALU.subtract, [o2slot, us], [sC["dT"]])
        for c in range(8):
            g = c // 2
            b = next_prj_bank()
            for k2 in range(2):
                mm(banks[b][:, 0:M], wpm[:, g, k2, (c % 2) * 128:(c % 2) * 128 + 128], dT[:, 2 * g + k2, 0:M],
                   k2 == 0, k2 == 1, [s_wpm, sC["dT"]], [s_bank[b]])
            stt(bbT[:, c, 0:M], banks[b][:, 0:M], colsT[:, c, r_ps(l):r_ps(l) + 1], zbT[:, c, 0:M], ALU.mult, ALU.mult,
                [s_bank[b], s_colsT, sC["zbT"]], [s_bbT])
        if tile.last:
            for ct in range(8):
                b = next_prj_bank()
                tr(banks[b][0:HIST, 0:128], ucarry[:, ct, :], ident_f[:, :], [s_ucarry, s_cf], [s_bank[b]])
                cp('act', hist_out[0:HIST, ct * 128:(ct + 1) * 128], banks[b][0:HIST, 0:128], [s_bank[b]],
                   [sC["dT"]])
            dst = pp_d[l, :, :] if tile.sample is None else ps_d[l, tile.sample, :, :]
            P.dma('sp', dst, hist_out[0:HIST, :], "hist_out", [sC["dT"]], ())

    ucarry = sb("ucarry", [128, 8, HIST], F32)
    s_ucarry = Slot("ucarry")

    def stage_B(l, tile):
        M, Lc, nch, q = tile.M, tile.Lc, tile.nch, tile.seq
        bi, bf_ = 2, 3
        for kt in range(KT):
            mm(banks[bi][0:4, 0:M], wg[:, kt, 0:4], hT[:, kt, 0:M], kt == 0, kt == KT - 1, [s_wg, s_hT], [s_bank[bi]])
        for kt in range(KT):
            mm(banks[bf_][0:4, 0:M], wg[:, kt, 4:8], hT[:, kt, 0:M], kt == 0, kt == KT - 1, [s_wg, s_hT], [s_bank[bf_]])
        act(g_ig[:, 0:M], banks[bi][0:4, 0:M], AF.Identity, [s_bank[bi], s_bif], [s_gig], bias=bif[:, l, 0:1])
        act(g_lf[:, 0:M], banks[bf_][0:4, 0:M], AF.Exp, [s_bank[bf_], s_nbff], [s_glf], bias=nbf_f[:, l:l + 1], scale=-1.0)
        act(g_lf[:, 0:M], g_lf[:, 0:M], AF.Ln, [s_glf, s_ones4], [s_glf], bias=ones4[:, 0:1])
        ts(g_lf[:, 0:M], g_lf[:, 0:M], -1.0, None, ALU.mult, None, [s_glf], [s_glf])
        if tile.first:
            if tile.sample is None:
                memset('dve', mcar[:], 0.0, [s_mcar])
            else:
                cp('dve', mcar[:], mst[:, l * NS + tile.sample:l * NS + tile.sample + 1], [s_mst], [s_mcar])
        ts(cend[:, 0:1], mcar[:], -1.0, None, ALU.mult, None, [s_mcar], [s_cend])
        P.add('dve', lambda e: e.tensor_tensor_scan(out=g_B[:, 0:M], data0=g_lf[:, 0:M], data1=g_lf[:, 0:M],
                                                    initial=0.0, op0=ALU.add, op1=ALU.add),
              [s_glf], [s_gB])
        P.add('dve', lambda e: e.tensor_tensor_scan(out=g_m[:, 0:M], data0=g_lf[:, 0:M], data1=g_ig[:, 0:M],
                                                    initial=mcar[:, 0:1], op0=ALU.add, op1=ALU.max),
              [s_glf, s_gig, s_mcar], [s_gm])
        stt(g_ig[:, 0:M], g_B[:, 0:M], -0.5, g_ig[:, 0:M], ALU.mult, ALU.add, [s_gig, s_gB], [s_gig])
        stt(g_B[:, 0:M], g_B[:, 0:M], 0.5, g_m[:, 0:M], ALU.mult, ALU.subtract, [s_gB, s_gm], [s_gB])
        cp('dve', mcar[:], g_m[:, M - 1:M], [s_gm], [s_mcar])
        cp('dve', cend[:, 1:nch + 1], g_c[:, Lc - 1:M:Lc], [s_gc], [s_cend])
        for h in range(NH):
            ts(cediag[:, h, 0:nch + 1], cend[:, 0:nch + 1], ident_f[0:4, h:h + 1], None, ALU.mult, None,
               [s_cend, s_cf], [s_cediag])
        cb = 4
        mm(banks[cb][:, 0:NH * (NCH + 1)], ones4[:, :], cediag[:].rearrange("p h j -> p (h j)"), True, True,
           [s_ones4, s_cediag], [s_bank[cb]])
        cp('act', CE[:].rearrange("p h j -> p (h j)"), banks[cb][:, 0:NH * (NCH + 1)], [s_bank[cb]], [s_CE])
        tb = 5
        for j in range(nch):
            for wi, (rowt, rslot) in enumerate([(g_g, s_gg), (g_c, s_gc), (g_m, s_gm)]):
                tr(banks[tb][0:Lc, (j * 3 + wi) * 4:(j * 3 + wi) * 4 + 4], rowt[0:4, j * Lc:(j + 1) * Lc],
                   ident_f[0:4, 0:4], [rslot, s_cf], [s_bank[tb]])
        cp('act', cols[0:Lc, 0:nch, :, :].rearrange("p a b c -> p (a b c)"), banks[tb][0:Lc, 0:nch * 12],
           [s_bank[tb]], [s_cols])
        CEp = CE[:, :, 0:nch].rearrange("p h j -> p j h")
        CEc = CE[:, :, 1:nch + 1].rearrange("p h j -> p j h")
        tt(tmpc[0:Lc, 0:nch, :], cols[0:Lc, 0:nch, 1, :], CEp[0:Lc], ALU.subtract, [s_cols, s_CE], [s_tmpc])
        act(acol[0:Lc, 0:nch, :], tmpc[0:Lc, 0:nch, :], AF.Exp, [s_tmpc], [s_acol])
        act(emcol[0:Lc, 0:nch, :], cols[0:Lc, 0:nch, 2, :], AF.Exp, [s_cols], [s_emcol], scale=-1.0)
        tt(tmpc[0:Lc, 0:nch, :], cols[0:Lc, 0:nch, 0, :], CEc[0:Lc], ALU.add, [s_cols, s_CE], [s_tmpc])
        act(wecol[0:Lc, 0:nch, :], tmpc[0:Lc, 0:nch, :], AF.Exp, [s_tmpc], [s_wecol])
        tt(tmpc[:, 0:nch, :], CEc, CEp, ALU.subtract, [s_CE], [s_tmpc])
        act(dccol[:, 0:nch, :], tmpc[:, 0:nch, :], AF.Exp, [s_tmpc], [s_dccol])

        if tile.first:
            if tile.sample is None:
                for h in range(NH):
                    for dt in range(4):
                        memset('dve', Cst[:, h, dt, :], 0.0, [s_Cst[h][dt]])
                memset('dve', nst[:], 0.0, [s_nst])
            else:
                s = tile.sample
                for h in range(NH):
                    P.dma('sp', Cst[:, h, :, :], stC_d[l, s, h].rearrange("(k p) e -> p k e", p=128), "Cld%d" % h,
                          (), s_Cst[h])
                cp('dve', nst[:].rearrange("p h d -> p (h d)"), colsT[:, :, r_n(l, s)], [s_colsT], [s_nst])

        for h in range(NH):
            head(l, tile, h)

        if tile.last:
            s = tile.sample
            for h in range(NH):
                dst = Cp_d[l, h] if s is None else Cs_d[l, s, h]
                P.dma('sp', dst.rearrange("(k p) e -> p k e", p=128), Cst[:, h, :, :], "Cst_out%d" % h, s_Cst[h], ())
            nb = 4
            tr(banks[nb][0:16, 0:128], nst[:].rearrange("p h d -> p (h d)"), ident_f[:, :], [s_nst, s_cf], [s_bank[nb]])
            sm = small[0:16, 1, :]
            cp('act', nout[0:16, :], banks[nb][0:16, 0:128], [s_bank[nb]], [s_nout])
            dstn = np_d[l] if s is None else ns_d[l, s]
            P.dma('sp', dstn, nout[0:16, :], "nout", [s_nout], ())
            dstm = mp_d[l:l + 1, :] if s is None else ms_d[l, s:s + 1, :]
            P.dma('sp', dstm.rearrange("o k -> k o"), mcar[:, 0:1], "mout", [s_mcar], ())

    nout = sb("nout", [16, 128], F32)
    s_nout = Slot("nout")

    def proj_fm(l, tile, off, dstT, dslot, evac):
        M = tile.M
        si, w = load_w(win_d[l, :, off:off + WCOLS], KT, WCOLS, ('fm', off))
        for ct in range(4):
            b = next_prj_bank()
            for kt in range(KT):
                mm(banks[b][:, 0:M], w[:, kt, ct * 128:(ct + 1) * 128], hT[:, kt, 0:M], kt == 0, kt == KT - 1,
                   si + [s_hT], [s_bank[b]])
            evac(ct, b)

    def proj_tm(l, tile, off, evac):
        M, Lc, nch = tile.M, tile.Lc, tile.nch
        si, w = load_w(win_d[l, :, off:off + WCOLS], KT, WCOLS, ('tm', off))
        for j in range(nch):
            b = next_prj_bank()
            for kt in range(KT):
                mm(banks[b][0:Lc, 0:DH], hT[:, kt, j * Lc:(j + 1) * Lc], w[:, kt, :], kt == 0, kt == KT - 1,
                   si + [s_hT], [s_bank[b]])
            evac(j, b)

    def head(l, tile, h):
        M, Lc, nch, q = tile.M, tile.Lc, tile.nch, tile.seq
        proj_fm(l, tile, OFF_Q + h * DH, qT, sB["qT"],
                lambda ct, b: cp('act', qT[:, ct, 0:M], banks[b][:, 0:M], [s_bank[b]], [sB["qT"]]))
        proj_fm(l, tile, OFF_K + h * DH, kT, sB["kT"],
                lambda ct, b: act(kT[:, ct, 0:M], banks[b][:, 0:M], AF.Identity, [s_bank[b]], [sB["kT"]],
                                  scale=float(DH) ** -0.5))
        proj_tm(l, tile, OFF_V + h * DH,
                lambda j, b: cp('dve', vtk[0:Lc, j, :], banks[b][0:Lc, 0:DH], [s_bank[b]], [sB["v"]]))
        proj_tm(l, tile, OFF_O + h * DH,
                lambda j, b: act(sgo[0:Lc, j, :], banks[b][0:Lc, 0:DH], AF.Sigmoid, [s_bank[b]], [sB["sgo"]]))
        proj_tm(l, tile, OFF_ZA + h * DH,
                lambda j, b: act(sza[0:Lc, j, :], banks[b][0:Lc, 0:DH], AF.Silu, [s_bank[b]], [sB["sza"]]))
        for dt in range(4):
            cp('act', Cbf[:, dt, :], Cst[:, h, dt, :], [s_Cst[h][dt]], [s_Cbf[dt]])
        cp('dve', nbf[:, h, :], nst[:, h, :], [s_nst], [s_nbf])

        for j in range(nch):
            i2 = j % 2
            cs = slice(j * Lc, (j + 1) * Lc)
            bS, bC, bA, bB = 2, 3, 4, 5
            for dt in range(4):
                mm(banks[bS][0:Lc, 0:Lc], kT[:, dt, cs], qT[:, dt, cs], dt == 0, dt == 3,
                   [sB["kT"], sB["qT"]], [s_bank[bS]])
            mm(banks[bC][0:Lc, 0:Lc], sel[0:4, h * 128:h * 128 + Lc], g_c[0:4, cs], True, False,
               [s_sel, s_gc], [s_bank[bC]])
            mm(banks[bC][0:Lc, 0:Lc], ident_f[0:Lc, 0:Lc], maskneg[0:Lc, 0:Lc], False, True,
               [s_cf], [s_bank[bC]])
            for dt in range(4):
                mm(banks[bA][0:Lc, 0:DH], qT[:, dt, cs], Cbf[:, dt, :], dt == 0, dt == 3,
                   [sB["qT"], s_Cbf[dt]], [s_bank[bA]])
            act(DTm[i2][0:Lc, 0:Lc], banks[bC][0:Lc, 0:Lc], AF.Exp, [s_bank[bC], s_cols], [sB2["DT"][i2]],
                bias=cols[0:Lc, j, 0, h:h + 1])
            tt(PTm[i2][0:Lc, 0:Lc], banks[bS][0:Lc, 0:Lc], DTm[i2][0:Lc, 0:Lc], ALU.mult,
               [s_bank[bS], sB2["DT"][i2]], [sB2["PT"][i2]])
            mm(banks[bB][0:Lc, 0:DH], PTm[i2][0:Lc, 0:Lc], vtk[0:Lc, j, :], True, True,
               [sB2["PT"][i2], sB["v"]], [s_bank[bB]])
            for dt in range(4):
                mm(banks[bS][0:Lc, 128:129], qT[:, dt, cs], nbf[:, h, dt:dt + 1], dt == 0, dt == 3,
                   [sB["qT"], s_nbf], [s_bank[bS]])
            mm(banks[bS][0:Lc, 129:130], PTm[i2][0:Lc, 0:Lc], ones_bf[0:Lc, 0:1], True, True,
               [sB2["PT"][i2], s_onesbf], [s_bank[bS]])
            sm = small[:, i2, :]
            ssl = s_small[i2]
            cp('act', sm[0:Lc, 0:2], banks[bS][0:Lc, 128:130], [s_bank[bS]], [ssl])
            stt(sm[0:Lc, 2:3], sm[0:Lc, 0:1], acol[0:Lc, j, h:h + 1], sm[0:Lc, 1:2], ALU.mult, ALU.add,
                [ssl, s_acol], [ssl])
            act(sm[0:Lc, 6:7], sm[0:Lc, 2:3], AF.Abs, [ssl], [ssl])
            tt(sm[0:Lc, 3:4], sm[0:Lc, 6:7], emcol[0:Lc, j, h:h + 1], ALU.max, [ssl, s_emcol], [ssl])
            P.add('dve', lambda e, sm=sm: e.reciprocal(out=sm[0:Lc, 4:5], in_=sm[0:Lc, 3:4]), [ssl], [ssl])
            tt(sm[0:Lc, 5:6], sm[0:Lc, 4:5], acol[0:Lc, j, h:h + 1], ALU.mult, [ssl, s_acol], [ssl])
            act(A2[i2][0:Lc, :], banks[bA][0:Lc, 0:DH], AF.Identity, [s_bank[bA], ssl], [sB2["A2"][i2]],
                scale=sm[0:Lc, 5:6])
            stt(hm[i2][0:Lc, :], banks[bB][0:Lc, 0:DH], sm[0:Lc, 4:5], A2[i2][0:Lc, :], ALU.mult, ALU.add,
                [s_bank[bB], ssl, sB2["A2"][i2]], [sB2["hm"][i2]])
            tt(hm[i2][0:Lc, :], hm[i2][0:Lc, :], sgo[0:Lc, j, :], ALU.mult, [sB2["hm"][i2], sB["sgo"]], [sB2["hm"][i2]])
            bs_ = bnst[:, i2, :]
            bsl = s_bnst[i2]
            P.add('dve', lambda e, bs_=bs_, x=hm[i2]: e.bn_stats(out=bs_[0:Lc, 0:6], in_=x[0:Lc, :]),
                  [sB2["hm"][i2]], [bsl])
            P.add('dve', lambda e, bs_=bs_: e.bn_aggr(out=bs_[0:Lc, 6:8], in_=bs_[0:Lc, 0:6]), [bsl], [bsl])
            act(sm[0:Lc, 8:9], bs_[0:Lc, 7:8], AF.Sqrt, [bsl, s_epsc], [ssl], bias=epsc[0:Lc, 0:1])
            P.add('dve', lambda e, sm=sm: e.reciprocal(out=sm[0:Lc, 9:10], in_=sm[0:Lc, 8:9]), [ssl], [ssl])
            ts(A2[i2][0:Lc, :], hm[i2][0:Lc, :], bs_[0:Lc, 6:7], sm[0:Lc, 9:10], ALU.subtract, ALU.mult,
               [sB2["hm"][i2], bsl, ssl], [sB2["A2"][i2]])
            tt(atok[i2][0:Lc, :], A2[i2][0:Lc, :], sza[0:Lc, j, :], ALU.mult, [sB2["A2"][i2], sB["sza"]],
               [sB2["atok"][i2]])
            bt = 6
            btv = banks[bt][:, :].bitcast(BF16)
            for i in range(4):
                tr(btv[:, i * 128:i * 128 + Lc], atok[i2][0:Lc, i * 128:(i + 1) * 128], ident_bf[0:Lc, 0:Lc],
                   [sB2["atok"][i2], s_identbf], [s_bank[bt]])
            for i in range(4):
                act(aT[:, h * 4 + i, cs], btv[:, i * 128:i * 128 + Lc], AF.Identity, [s_bank[bt], s_colsT], [s_aT],
                    scale=colsT[:, h * 4 + i, r_hn(l):r_hn(l) + 1])
            bk = 7
            bkv = banks[bk][:, :].bitcast(BF16)
            for dt in range(4):
                tr(bkv[0:Lc, dt * 128:(dt + 1) * 128], kT[:, dt, cs], ident_bf[:, :], [sB["kT"], s_identbf],
                   [s_bank[bk]])
            act(kw[i2][0:Lc, :], bkv[0:Lc, 0:DH], AF.Identity, [s_bank[bk], s_wecol], [sB2["kw"][i2]],
                scale=wecol[0:Lc, j, h:h + 1])
            for dt in range(4):
                bu = 0 + (dt % 2)
                mm(banks[bu][:, 0:DH], kw[i2][0:Lc, dt * 128:(dt + 1) * 128], vtk[0:Lc, j, :], True, True,
                   [sB2["kw"][i2], sB["v"]], [s_bank[bu]])
                stt(Cst[:, h, dt, :], Cst[:, h, dt, :], dccol[:, j, h:h + 1], banks[bu][:, 0:DH], ALU.mult, ALU.add,
                    [s_Cst[h][dt], s_dccol, s_bank[bu]], [s_Cst[h][dt]])
                if j < nch - 1:
                    cp('act', Cbf[:, dt, :], Cst[:, h, dt, :], [s_Cst[h][dt]], [s_Cbf[dt]])
            for dt in range(4):
                mm(banks[bC][:, 128 + dt:129 + dt], kw[i2][0:Lc, dt * 128:(dt + 1) * 128], ones_bf[0:Lc, 0:1],
                   True, True, [sB2["kw"][i2], s_onesbf], [s_bank[bC]])
            stt(nst[:, h, :], nst[:, h, :], dccol[:, j, h:h + 1], banks[bC][:, 128:132], ALU.mult, ALU.add,
                [s_nst, s_dccol, s_bank[bC]], [s_nst])
            if j < nch - 1:
                cp('dve', nbf[:, h, :], nst[:, h, :], [s_nst], [s_nbf])

    def stage_D(l, tile):
        M, Lc, nch, q = tile.M, tile.Lc, tile.nch, tile.seq
        xsl = xres_slot(q)
        HC = WCOLS // 2
        for jb in range(D // HC):
            c0 = jb * HC
            sa, wga = load_w(win_d[l, :, OFF_GA + c0:OFF_GA + c0 + HC], KT, HC, ('ga', jb))
            sb_, wgb = load_w(win_d[l, :, OFF_GB + c0:OFF_GB + c0 + HC], KT, HC, ('gb', jb))
            sA_, wA = load_w(wa_d[l, :, c0:c0 + HC], KT, HC, ('wa', jb))
            sB_, wB = load_w(wb_d[l, :, c0:c0 + HC], 8, HC, ('wb', jb))
            for ct in range(2):
                jt = jb * 2 + ct
                csl = slice(ct * 128, (ct + 1) * 128)
                i2 = jt % 2
                b1, b2, b3, b4 = 2, 3, 4, 5
                for kt in range(KT):
                    mm(banks[b1][:, 0:M], wga[:, kt, csl], hT[:, kt, 0:M], kt == 0, kt == KT - 1,
                       sa + [s_hT], [s_bank[b1]])
                act(sg[i2][:, 0:M], banks[b1][:, 0:M], AF.Sigmoid, [s_bank[b1]], [sD["sg%d" % i2]])
                for kt in range(KT):
                    mm(banks[b2][:, 0:M], wgb[:, kt, csl], hT[:, kt, 0:M], kt == 0, kt == KT - 1,
                       sb_ + [s_hT], [s_bank[b2]])
                act(sg[2 + i2][:, 0:M], banks[b2][:, 0:M], AF.Sigmoid, [s_bank[b2]], [sD["sg%d" % (2 + i2)]])
                for kt in range(KT):
                    mm(banks[b3][:, 0:M], wA[:, kt, csl], aT[:, kt, 0:M], kt == 0, kt == KT - 1,
                       sA_ + [s_aT], [s_bank[b3]])
                tt(t12[0][:, 0:M], banks[b3][:, 0:M], sg[i2][:, 0:M], ALU.mult, [s_bank[b3], sD["sg%d" % i2]],
                   [sD["t12_0"]])
                for kt in range(8):
                    mm(banks[b4][:, 0:M], wB[:, kt, csl], bbT[:, kt, 0:M], kt == 0, kt == 7,
                       sB_ + [s_bbT], [s_bank[b4]])
                tt(t12[1][:, 0:M], banks[b4][:, 0:M], sg[2 + i2][:, 0:M], ALU.mult,
                   [s_bank[b4], sD["sg%d" % (2 + i2)]], [sD["t12_1"]])
                tt(mgT[:, jt, 0:M], t12[0][:, 0:M], t12[1][:, 0:M], ALU.add, [sD["t12_0"], sD["t12_1"]], [sD["mgT"]])
        for jb in range(D // WCOLS):
            c0 = jb * WCOLS
            so, wo = load_w(wo_d[l, :, c0:c0 + WCOLS], KT, WCOLS, ('wo', jb))
            for ct in range(4):
                jt = jb * 4 + ct
                csl = slice(ct * 128, (ct + 1) * 128)
                i2 = jt % 2
                b = next_prj_bank()
                P.dma('sp', xin[i2][:, 0:M], tile.xres(jt), "xin%d" % i2, xsl + pre_slots, [sD["xin%d" % i2]])
                for kt in range(KT):
                    mm(banks[b][:, 0:M], wo[:, kt, csl], mgT[:, kt, 0:M], kt == 0, kt == KT - 1,
                       so + [sD["mgT"]], [s_bank[b]])
                stt(xout[i2][:, 0:M], banks[b][:, 0:M], modT[:, 2 * KT + jt, q:q + 1], xin[i2][:, 0:M], ALU.mult, ALU.add,
                    [s_bank[b], s_modT, sD["xin%d" % i2]], [sD["xout%d" % i2]])
                P.dma('sp', tile.xres(jt), xout[i2][:, 0:M], "xout%d" % i2, [sD["xout%d" % i2]], [xsl[i2]])

    def final_tile(tile):
        M, Lc, nch, q = tile.M, tile.Lc, tile.nch, tile.seq
        xsl = xres_slot(q)

        def load_x(kt, buf, slot):
            P.dma('sp', buf[:, 0:M], tile.xres(kt), "ld_" + slot.name, xsl + pre_slots, [slot])

        rms_stats(tile, load_x, fxa, [sF["fxa0"], sF["fxa1"]], fsq, [sF["fsq0"], sF["fsq1"]])
        for j in range(nch):
            yo = j % 2
            cs = slice(j * Lc, (j + 1) * Lc)
            for g in range(4):
                b = next_prj_bank()
                for i in range(4):
                    kt = g * 4 + i
                    i2 = kt % 2
                    P.dma('sp', fxa[i2][:, 0:Lc], tile.xres(kt)[:, cs], "ld_" + sF["fxa%d" % i2].name,
                          xsl + pre_slots, [sF["fxa%d" % i2]])
                    stt(fta[i2][:, 0:Lc], fxa[i2][:, 0:Lc], colsT[:, kt, r_fin:r_fin + 1], rstd_bc[:, cs],
                        ALU.mult, ALU.mult, [sF["fxa%d" % i2], s_colsT, s_rstd], [sF["fta%d" % i2]])
                    tr(banks[b][0:Lc, i * 128:(i + 1) * 128], fta[i2][:, 0:Lc], ident_f[:, :],
                       [sF["fta%d" % i2], s_cf], [s_bank[b]])
                cp('act' if g % 2 == 0 else 'dve', ytok[yo][0:Lc, g * 512:(g + 1) * 512], banks[b][0:Lc, :],
                   [s_bank[b]], [sF["ytok%d" % yo]])
            if tile.sample is None:
                dst = yp_d[tile.t0 + j * Lc:tile.t0 + (j + 1) * Lc, :]
            else:
                dst = ys_d[tile.sample, :, :]
            P.dma('sp', dst, ytok[yo][0:Lc, :], "ytok%d" % yo, [sF["ytok%d" % yo]], ())

    for l in range(L):
        layer(l)
    for tile in tiles:
        final_tile(tile)

    P.emit()
    return nc


_CACHE = {}


def _consts():
    cf = np.zeros((128, 320), np.float32)
    cf[:, 0:128] = np.eye(128, dtype=np.float32)
    s = np.arange(128)[:, None]
    t = np.arange(128)[None, :]
    cf[:, 128:256] = np.where(s <= t, 0.0, -30000.0).astype(np.float32)
    for g, w in enumerate((2, 4, 8, 16)):
        for tt_ in range(16):
            cf[:, 256 + g * 16 + tt_] = 1.0 / min(w, tt_ + 1)
    sel = np.zeros((4, 4, 128), np.float32)
    for h in range(4):
        sel[h, h, :] = 1.0
    return cf, sel.reshape(4, 512)


def kernel(x_prompt, x_sample, c_prompt, c_sample, state_C, state_n, state_m, state_pool,
           norm_w, w_ada, b_ada, w_in, b_if, head_norm_w, w_pool_mix, pool_scale,
           w_branch_a, w_branch_b, w_out, final_norm_w):
    f = lambda a: np.ascontiguousarray(np.asarray(a, dtype=np.float32))
    x_prompt, x_sample, c_prompt, c_sample = f(x_prompt), f(x_sample), f(c_prompt), f(c_sample)
    state_C, state_n, state_m, state_pool = f(state_C), f(state_n), f(state_m), f(state_pool)
    norm_w, w_ada, b_ada, w_in, b_if = f(norm_w), f(w_ada), f(b_ada), f(w_in), f(b_if)
    head_norm_w, w_pool_mix, pool_scale = f(head_norm_w), f(w_pool_mix), f(pool_scale)
    w_branch_a, w_branch_b, w_out, final_norm_w = f(w_branch_a), f(w_branch_b), f(w_out), f(final_norm_w)

    B, T, _ = x_prompt.shape
    SB, SL, _ = x_sample.shape
    L = w_in.shape[0]
    NCORES = 8
    NS = SB // B
    assert B * 2 == NCORES and SB == B * NS
    key = (T, NS, SL, L)
    if key not in _CACHE:
        _CACHE[key] = build_program(T, NS, SL, L)
    nc = _CACHE[key]
    NSEQ = 1 + NS
    NR = 6 * L + 1 + NSEQ + L * NS
    cf, sel = _consts()
    real = [0, 2, 4, 6]
    zcache = {}

    def zeros_like(a):
        k = (a.shape, a.dtype.str)
        if k not in zcache:
            zcache[k] = np.zeros(a.shape, a.dtype)
        return zcache[k]

    in_maps = []
    for core in range(NCORES):
        c = core // 2
        rows = np.zeros((NR, D), np.float32)
        for l in range(L):
            rows[l] = norm_w[l]
            rows[L + 3 * l:L + 3 * l + 3] = b_ada[l].reshape(3, D)
            rows[4 * L + l] = head_norm_w[l]
            rows[5 * L + l, 0:DB] = pool_scale[l]
        rows[6 * L] = final_norm_w
        rows[6 * L + 1] = c_prompt[c]
        for s in range(NS):
            rows[6 * L + 2 + s] = c_sample[c * NS + s]
        for l in range(L):
            for s in range(NS):
                rows[6 * L + 1 + NSEQ + l * NS + s] = state_n[l, c * NS + s].reshape(-1)
        m = {
            "xp": x_prompt[c], "xs": x_sample[c * NS:(c + 1) * NS], "rows": rows, "bif": b_if,
            "st_m": state_m[:, c * NS:(c + 1) * NS], "st_C": state_C[:, c * NS:(c + 1) * NS],
            "st_pool": state_pool[:, c * NS:(c + 1) * NS], "w_ada": w_ada, "w_in": w_in, "w_pm": w_pool_mix,
            "w_a": w_branch_a, "w_b": w_branch_b, "w_o": w_out, "cf": cf, "sel": sel,
        }
        m = {k_: np.ascontiguousarray(v_) for k_, v_ in m.items()}
        if core not in real:
            m = {k_: zeros_like(v_) for k_, v_ in m.items()}
        in_maps.append(m)
    res = run_bass_kernel_spmd(nc, in_maps, core_ids=list(range(NCORES)))
    R = [res.results[i] for i in real]
    y_prompt = np.stack([R[c]["yp"] for c in range(B)])
    y_sample = np.concatenate([R[c]["ys"] for c in range(B)], axis=0)
    Cp = np.stack([R[c]["Cp"] for c in range(B)], axis=1)
    np_ = np.stack([R[c]["np"].reshape(L, NH, DH) for c in range(B)], axis=1)
    mp = np.stack([R[c]["mp"] for c in range(B)], axis=1)
    pp = np.stack([R[c]["pp"] for c in range(B)], axis=1)
    Cs = np.concatenate([R[c]["Cs"] for c in range(B)], axis=1)
    ns = np.concatenate([R[c]["ns"].reshape(L, NS, NH, DH) for c in range(B)], axis=1)
    ms = np.concatenate([R[c]["ms"] for c in range(B)], axis=1)
    ps = np.concatenate([R[c]["ps"] for c in range(B)], axis=1)
    return (y_prompt.astype(np.float32), y_sample.astype(np.float32), Cp.astype(np.float32),
            np_.astype(np.float32), mp.astype(np.float32), pp.astype(np.float32), Cs.astype(np.float32),
            ns.astype(np.float32), ms.astype(np.float32), ps.astype(np.float32))
```

```python
import numpy as np
import concourse.bass as bass
import concourse.mybir as mybir
from concourse.bass_utils import run_bass_kernel_spmd

F32 = mybir.dt.float32
BF16 = mybir.dt.bfloat16
AF = mybir.ActivationFunctionType
ALU = mybir.AluOpType

D = 2048
KT = 16
NH = 4
DH = 512
DB = 1024
HIST = 15
NIN = 16392
OFF_Q, OFF_K, OFF_V, OFF_O, OFF_ZA, OFF_IF, OFF_U, OFF_ZB, OFF_GA, OFF_GB = (
    0, 2048, 4096, 6144, 8192, 10240, 10248, 11272, 12296, 14344)
EPS = 1e-6
WCOLS = 512
NSLOT = 4
ENGS = ['pe', 'act', 'dve', 'pool', 'sp']


class Slot:
    __slots__ = ('name', 'excl', 'last_w', 'readers', 'alias')

    def __init__(self, name, excl=False):
        self.name = name
        self.excl = excl
        self.last_w = None
        self.readers = {}
        self.alias = [self]


def alias_group(slots):
    for s in slots:
        s.alias = list(slots)


class Op:
    __slots__ = ('eng', 'fn', 'idx', 'is_dma', 'dkey', 'dval', 'need_inc', 'semval', 'waits')


class Prog:
    def __init__(self, nc):
        self.nc = nc
        self.ops = {e: [] for e in ENGS}
        self.eng_sem = {e: nc.alloc_semaphore(name="sem_" + e) for e in ENGS}
        self.dma_sem = {}
        self.dma_cnt = {}
        self.seen = {e: {} for e in ENGS}

    def add(self, eng, fn, reads=(), writes=(), dma_key=None):
        op = Op()
        op.eng = eng
        op.fn = fn
        op.idx = len(self.ops[eng])
        op.is_dma = dma_key is not None
        op.dkey = dma_key
        op.need_inc = False
        op.semval = 0
        op.dval = 0
        if op.is_dma:
            if dma_key not in self.dma_sem:
                self.dma_sem[dma_key] = self.nc.alloc_semaphore(name="dsem_%d" % len(self.dma_sem))
                self.dma_cnt[dma_key] = 0
            self.dma_cnt[dma_key] += 16
            op.dval = self.dma_cnt[dma_key]
        deps = []
        for s in reads:
            for a in s.alias:
                if a.last_w is not None:
                    deps.append(a.last_w)
                if a.excl:
                    for r in a.readers.values():
                        if r.eng != eng:
                            deps.append(r)
        for s in writes:
            for a in s.alias:
                if a.last_w is not None:
                    deps.append(a.last_w)
                deps.extend(a.readers.values())
        waits = {}
        seen = self.seen[eng]
        for d in deps:
            if d is op:
                continue
            if d.is_dma:
                key = ('d', d.dkey)
                val = d.dval
            else:
                if d.eng == eng and not op.is_dma:
                    if eng == 'pe':
                        continue
                    if op.idx - d.idx > 3:
                        continue
                key = ('e', d.eng)
                val = d.idx
            if seen.get(key, -1) >= val:
                continue
            if key not in waits or waits[key].__getattribute__('dval' if d.is_dma else 'idx') < val:
                waits[key] = d
        for key, d in waits.items():
            seen[key] = d.dval if d.is_dma else d.idx
            d.need_inc = True
        op.waits = list(waits.values())
        rkey = ('d', dma_key) if op.is_dma else ('e', eng)
        for s in reads:
            s.readers[rkey] = op
        for s in writes:
            s.last_w = op
            s.readers = {}
        self.ops[eng].append(op)
        return op

    def dma(self, eng, out, in_, key, reads=(), writes=()):
        return self.add(eng, lambda e: e.dma_start(out=out, in_=in_), reads, writes, dma_key=key)

    def emit(self):
        nc = self.nc
        for e in ENGS:
            c = 0
            for op in self.ops[e]:
                if (not op.is_dma) and op.need_inc:
                    c += 1
                    op.semval = c
        final_waits = [(self.dma_sem[k], v) for k, v in self.dma_cnt.items()]

        def run(e, eng):
            for op in self.ops[e]:
                for d in op.waits:
                    if d.is_dma:
                        eng.wait_ge(self.dma_sem[d.dkey], d.dval)
                    else:
                        eng.wait_ge(self.eng_sem[d.eng], d.semval)
                ins = op.fn(eng)
                if op.is_dma:
                    ins.then_inc(self.dma_sem[op.dkey], 16)
                elif op.need_inc:
                    ins.then_inc(self.eng_sem[e], 1)
            if e == 'sp':
                for sem, v in final_waits:
                    eng.wait_ge(sem, v)

        with nc.allow_non_contiguous_dma(reason="tiny strided state/bias transfers"), nc.Block() as block:
            @block.tensor
            def _(eng):
                run('pe', eng)

            @block.scalar
            def _(eng):
                run('act', eng)

            @block.vector
            def _(eng):
                run('dve', eng)

            @block.gpsimd
            def _(eng):
                run('pool', eng)

            @block.sync
            def _(eng):
                run('sp', eng)


def build_program(T, NS, SL, L, MT=512):
    nc = bass.Bass("TRN2", target_bir_lowering=False)
    P = Prog(nc)
    NSEQ = 1 + NS
    NR = 6 * L + 1 + NSEQ + L * NS
    assert NR <= 128 and T % MT == 0 and MT % 128 == 0

    def r_norm(l): return l
    def r_ada(l, w): return L + 3 * l + w
    def r_hn(l): return 4 * L + l
    def r_ps(l): return 5 * L + l
    r_fin = 6 * L
    def r_c(q): return 6 * L + 1 + q
    def r_n(l, s): return 6 * L + 1 + NSEQ + l * NS + s

    def din(name, shape):
        return nc.dram_tensor(name, list(shape), F32, kind="ExternalInput").ap()

    def dout(name, shape):
        return nc.dram_tensor(name, list(shape), F32, kind="ExternalOutput").ap()

    xp = din("xp", [T, D])
    xs = din("xs", [NS, SL, D])
    rows_d = din("rows", [NR, D])
    bif_d = din("bif", [L, 8])
    stm_d = din("st_m", [L, NS, NH])
    stC_d = din("st_C", [L, NS, NH, DH, DH])
    stP_d = din("st_pool", [L, NS, HIST, DB])
    wada_d = din("w_ada", [L, D, 3 * D])
    win_d = din("w_in", [L, D, NIN])
    wpm_d = din("w_pm", [L, 4, 256, 256])
    wa_d = din("w_a", [L, D, D])
    wb_d = din("w_b", [L, DB, D])
    wo_d = din("w_o", [L, D, D])
    cf_d = din("cf", [128, 320])
    sel_d = din("sel", [4, 4 * 128])

    yp_d = dout("yp", [T, D])
    ys_d = dout("ys", [NS, SL, D])
    Cp_d = dout("Cp", [L, NH, DH, DH])
    np_d = dout("np", [L, NH * 4, 128])
    mp_d = dout("mp", [L, NH])
    pp_d = dout("pp", [L, HIST, DB])
    Cs_d = dout("Cs", [L, NS, NH, DH, DH])
    ns_d = dout("ns", [L, NS, NH * 4, 128])
    ms_d = dout("ms", [L, NS, NH])
    ps_d = dout("ps", [L, NS, HIST, DB])

    xres_p = nc.dram_tensor("xres_p", [KT, 128, T], F32, kind="Internal").ap()
    xres_s = nc.dram_tensor("xres_s", [NS, KT, 128, SL], F32, kind="Internal").ap()

    def sb(name, shape, dt):
        return nc.alloc_sbuf_tensor("sb_" + name, list(shape), dt)

    hT = sb("hT", [128, KT, MT], BF16); s_hT = Slot("hT")
    aT = sb("aT", [128, KT, MT], BF16); s_aT = Slot("aT")
    bbT = sb("bbT", [128, 8, MT], BF16); s_bbT = Slot("bbT")
    Cst = sb("Cst", [128, NH, 4, DH], F32)
    s_Cst = [[Slot("Cst%d_%d" % (h, dt)) for dt in range(4)] for h in range(NH)]
    Cbf = sb("Cbf", [128, 4, DH], BF16)
    s_Cbf = [Slot("Cbf%d" % dt) for dt in range(4)]
    nst = sb("nst", [128, NH, 4], F32); s_nst = Slot("nst")
    nbf = sb("nbf", [128, NH, 4], BF16); s_nbf = Slot("nbf")
    ring = [sb("ring%d" % i, [128, KT, WCOLS], BF16) for i in range(NSLOT)]
    wg = sb("wg", [128, KT, 8], BF16); s_wg = Slot("wg")
    wpm = sb("wpm", [128, 4, 2, 256], BF16); s_wpm = Slot("wpm")
    cf = sb("cf", [128, 320], F32); s_cf = Slot("cf")
    ident_f = cf[:, 0:128]
    maskneg = cf[:, 128:256]
    invcnt = cf[:, 256:320]
    sel = sb("sel", [4, 4 * 128], F32); s_sel = Slot("sel")
    ident_bf = sb("ident_bf", [128, 128], BF16); s_identbf = Slot("identbf")
    ones_bf = sb("ones_bf", [128, 128], BF16); s_onesbf = Slot("onesbf")
    ones4 = sb("ones4", [4, 128], F32); s_ones4 = Slot("ones4")
    epsc = sb("epsc", [128, 1], F32); s_epsc = Slot("epsc")
    colsT = sb("colsT", [128, KT, NR], F32); s_colsT = Slot("colsT")
    csT = sb("csT", [128, KT, NSEQ], BF16); s_csT = Slot("csT")
    bif = sb("bif", [4, L, 2], F32); s_bif = Slot("bif")
    nbf_f = sb("nbf_f", [4, L], F32); s_nbff = Slot("nbff")
    mst = sb("mst", [4, L * NS], F32); s_mst = Slot("mst")
    mcar = sb("mcar", [4, 1], F32); s_mcar = Slot("mcar")
    modT = sb("modT", [128, 48, NSEQ], F32); s_modT = Slot("modT")
    AmodT = sb("AmodT", [128, KT, NSEQ], F32); s_AmodT = Slot("AmodT")
    g_ig = sb("g_ig", [4, MT], F32); s_gig = Slot("g_ig")
    g_lf = sb("g_lf", [4, MT], F32); s_glf = Slot("g_lf")
    g_B = sb("g_B", [4, MT], F32); s_gB = Slot("g_B")
    g_m = sb("g_m", [4, MT], F32); s_gm = Slot("g_m")
    g_g, g_c, s_gg, s_gc = g_ig, g_B, s_gig, s_gB
    NCH = MT // 128
    cend = sb("cend", [4, NCH + 1], F32); s_cend = Slot("cend")
    cediag = sb("cediag", [4, NH, NCH + 1], F32); s_cediag = Slot("cediag")
    CE = sb("CE", [128, NH, NCH + 1], F32); s_CE = Slot("CE")
    cols = sb("cols", [128, NCH, 3, NH], F32); s_cols = Slot("cols")
    acol = sb("acol", [128, NCH, NH], F32); s_acol = Slot("acol")
    emcol = sb("emcol", [128, NCH, NH], F32); s_emcol = Slot("emcol")
    wecol = sb("wecol", [128, NCH, NH], F32); s_wecol = Slot("wecol")
    dccol = sb("dccol", [128, NCH, NH], F32); s_dccol = Slot("dccol")
    tmpc = sb("tmpc", [128, NCH, NH], F32); s_tmpc = Slot("tmpc")
    small = sb("small", [128, 2, 16], F32)
    s_small = [Slot("small0"), Slot("small1")]
    bnst = sb("bnst", [128, 2, 8], F32)
    s_bnst = [Slot("bnst0"), Slot("bnst1")]
    rstd_bc = sb("rstd_bc", [128, MT], F32); s_rstd = Slot("rstd")

    UN = 18432
    U = sb("U", [128, UN], BF16)

    class Reg:
        def __init__(self):
            self.off = 0

        def take(self, name, nelem, dt):
            n16 = nelem * (2 if dt == F32 else 1)
            o = self.off
            self.off += n16
            assert self.off <= UN, (name, self.off)
            ap = U[:, o:o + n16]
            if dt == F32:
                ap = ap.bitcast(F32)
            return ap

    s_U = Slot("U_phase")

    rb = Reg()
    qT = rb.take("qT", 4 * MT, BF16).rearrange("p (a b) -> p a b", a=4)
    kT = rb.take("kT", 4 * MT, BF16).rearrange("p (a b) -> p a b", a=4)
    vtk = rb.take("v", NCH * DH, BF16).rearrange("p (a b) -> p a b", a=NCH)
    sgo = rb.take("sgo", NCH * DH, BF16).rearrange("p (a b) -> p a b", a=NCH)
    sza = rb.take("sza", NCH * DH, BF16).rearrange("p (a b) -> p a b", a=NCH)
    A2 = [rb.take("A2_%d" % i, DH, F32) for i in range(2)]
    hm = [rb.take("hm_%d" % i, DH, F32) for i in range(2)]
    atok = [rb.take("atok_%d" % i, DH, BF16) for i in range(2)]
    kw = [rb.take("kw_%d" % i, DH, BF16) for i in range(2)]
    DTm = [rb.take("DT_%d" % i, 128, F32) for i in range(2)]
    PTm = [rb.take("PT_%d" % i, 128, BF16) for i in range(2)]
    sB = {n: Slot("B_" + n) for n in ["qT", "kT", "v", "sgo", "sza"]}
    sB2 = {n: [Slot("B_%s%d" % (n, i)) for i in range(2)] for n in ["A2", "hm", "atok", "kw", "DT", "PT"]}
    rc = Reg()
    UW = HIST + MT
    uT2 = [rc.take("uT%d" % i, UW, F32) for i in range(2)]
    zbT = rc.take("zbT", 8 * MT, BF16).rearrange("p (a b) -> p a b", a=8)
    dT = rc.take("dT", 8 * MT, BF16).rearrange("p (a b) -> p a b", a=8)
    ptmp = [rc.take("ptmp%d" % i, UW, F32) for i in range(2)]
    sC = {n: Slot("C_" + n) for n in ["uT0", "uT1", "zbT", "dT", "ptmp0", "ptmp1"]}
    hist_in = zbT.rearrange("p a b -> p (a b)")[:, 0:2 * DB].bitcast(F32)
    hist_out = dT.rearrange("p a b -> p (a b)")[:, 0:2 * DB].bitcast(F32)
    rd = Reg()
    mgT = rd.take("mgT", KT * MT, BF16).rearrange("p (a b) -> p a b", a=KT)
    sg = [rd.take("sg%d" % i, MT, F32) for i in range(4)]
    t12 = [rd.take("t12_%d" % i, MT, F32) for i in range(2)]
    xin = [rd.take("xin%d" % i, MT, F32) for i in range(2)]
    xout = [rd.take("xout%d" % i, MT, F32) for i in range(2)]
    sD = {"mgT": Slot("D_mgT")}
    for i in range(4):
        sD["sg%d" % i] = Slot("D_sg%d" % i)
    for i in range(2):
        sD["t12_%d" % i] = Slot("D_t12_%d" % i)
        sD["xin%d" % i] = Slot("D_xin%d" % i)
        sD["xout%d" % i] = Slot("D_xout%d" % i)
    ra = Reg()
    xa = [ra.take("xa%d" % i, MT, F32) for i in range(2)]
    sqb = [ra.take("sqb%d" % i, MT, BF16) for i in range(2)]
    ta = [ra.take("ta%d" % i, MT, F32) for i in range(2)]
    sA = {}
    for i in range(2):
        sA["xa%d" % i] = Slot("A_xa%d" % i)
        sA["sqb%d" % i] = Slot("A_sqb%d" % i)
        sA["ta%d" % i] = Slot("A_ta%d" % i)
    ri = Reg()
    rows_sb = ri.take("rows", D, F32)
    xtok = ri.take("xtok", D, F32)
    xo4 = [ri.take("xo4_%d" % i, 512, F32).rearrange("p (a b) -> p a b", a=4) for i in range(2)]
    sI = {"rows": Slot("I_rows"), "xtok": Slot("I_xtok"), "xo4_0": Slot("I_xo4_0"), "xo4_1": Slot("I_xo4_1")}
    rf = Reg()
    fxa = [rf.take("fxa%d" % i, MT, F32) for i in range(2)]
    fsq = [rf.take("fsq%d" % i, MT, BF16) for i in range(2)]
    fta = [rf.take("fta%d" % i, MT, F32) for i in range(2)]
    ytok = [rf.take("ytok%d" % i, D, F32) for i in range(2)]
    sF = {}
    for i in range(2):
        for n in ["fxa", "fsq", "fta", "ytok"]:
            sF["%s%d" % (n, i)] = Slot("F_%s%d" % (n, i))
    allU = (list(sB.values()) + [x for v in sB2.values() for x in v] + list(sC.values()) +
            list(sD.values()) + list(sA.values()) + list(sI.values()) + list(sF.values()))
    phases = [list(sB.values()) + [x for v in sB2.values() for x in v], list(sC.values()),
              list(sD.values()), list(sA.values()), list(sI.values()), list(sF.values())]
    for pi, ph in enumerate(phases):
        others = [s for pj, q in enumerate(phases) if pj != pi for s in q]
        for s in ph:
            s.alias = [s] + others

    banks = [nc.alloc_psum_tensor("psb%d" % i, [128, 512], F32) for i in range(8)]
    s_bank = [Slot("bank%d" % i, excl=True) for i in range(8)]
    prj_rot = [0]

    def next_prj_bank():
        b = prj_rot[0]
        prj_rot[0] = (b + 1) % 2
        return b

    def mm(out, lhsT, rhs, start, stop, reads, writes):
        P.add('pe', lambda e: e.matmul(out, lhsT, rhs, start=start, stop=stop), reads, writes)

    def tr(out, in_, ident, reads, writes):
        P.add('pe', lambda e: e.transpose(out, in_, ident), reads, writes)

    def act(out, in_, func, reads, writes, bias=None, scale=None):
        kw_ = {}
        if bias is not None:
            kw_['bias'] = bias
        if scale is not None:
            kw_['scale'] = scale
        P.add('act', lambda e: e.activation(out=out, in_=in_, func=func, **kw_), reads, writes)

    def tt(out, in0, in1, op, reads, writes, eng='dve'):
        P.add(eng, lambda e: e.tensor_tensor(out=out, in0=in0, in1=in1, op=op), reads, writes)

    def ts(out, in0, s1, s2, op0, op1, reads, writes, eng='dve'):
        if op1 is None:
            P.add(eng, lambda e: e.tensor_scalar(out=out, in0=in0, scalar1=s1, scalar2=None, op0=op0), reads, writes)
        else:
            P.add(eng, lambda e: e.tensor_scalar(out=out, in0=in0, scalar1=s1, scalar2=s2, op0=op0, op1=op1),
                  reads, writes)

    def stt(out, in0, scalar, in1, op0, op1, reads, writes):
        P.add('dve', lambda e: e.scalar_tensor_tensor(out=out, in0=in0, scalar=scalar, in1=in1, op0=op0, op1=op1),
              reads, writes)

    def cp(eng, out, in_, reads, writes):
        if eng == 'act':
            P.add('act', lambda e: e.activation(out=out, in_=in_, func=AF.Identity), reads, writes)
        else:
            P.add(eng, lambda e: e.tensor_copy(out=out, in_=in_), reads, writes)

    def memset(eng, ap, val, writes):
        P.add(eng, lambda e: e.memset(ap, val), (), writes)

    ring_ctr = [0]
    wcache_idx = {}
    NCACHE = 64
    wcache = nc.dram_tensor("wcache", [NCACHE, 128, KT * WCOLS], BF16, kind="Internal").ap()
    s_wcache = [Slot("wcache%d" % i) for i in range(NCACHE)]
    s_half = [Slot("ringh%d" % i) for i in range(2 * NSLOT)]

    def load_w(src_rows_ap, nk, ncols, blk_id=None):
        if ncols == WCOLS:
            if ring_ctr[0] % 2:
                ring_ctr[0] += 1
            h0 = ring_ctr[0] % (2 * NSLOT)
            ring_ctr[0] += 2
            hs = [s_half[h0], s_half[h0 + 1]]
            dst = ring[h0 // 2][:, 0:nk, :]
        else:
            assert ncols == WCOLS // 2
            h0 = ring_ctr[0] % (2 * NSLOT)
            ring_ctr[0] += 1
            hs = [s_half[h0]]
            dst = ring[h0 // 2][:, 0:nk, (h0 % 2) * ncols:(h0 % 2 + 1) * ncols]
        key = "ring%d" % h0
        if blk_id is not None and blk_id in wcache_idx:
            ci = wcache_idx[blk_id]
            P.dma('sp', dst, wcache[ci, :, 0:nk * ncols].rearrange("p (k c) -> p k c", k=nk), key,
                  [s_wcache[ci]], hs)
        else:
            src = src_rows_ap.rearrange("(k p) c -> p k c", p=128)
            P.dma('pool', dst, src, key, (), hs)
            if blk_id is not None:
                ci = len(wcache_idx)
                assert ci < NCACHE
                wcache_idx[blk_id] = ci
                P.dma('pool', wcache[ci, :, 0:nk * ncols].rearrange("p (k c) -> p k c", k=nk), dst, "wst%d" % h0,
                      hs, [s_wcache[ci]])
        return hs, dst

    P.dma('sp', cf[:], cf_d[:, :], "cf", (), [s_cf])
    P.dma('sp', sel[:], sel_d[:, :], "sel", (), [s_sel])
    P.dma('sp', rows_sb[0:NR, :], rows_d[:, :], "rows", (), [sI["rows"]])
    P.dma('sp', bif[:], bif_d.rearrange("l (w k) -> k l w", w=2), "bif", (), [s_bif])
    P.dma('sp', mst[:], stm_d.rearrange("l s k -> k (l s)"), "mst", (), [s_mst])
    memset('dve', ones_bf[:], 1.0, [s_onesbf])
    memset('dve', ones4[:], 1.0, [s_ones4])
    memset('dve', epsc[:], EPS, [s_epsc])
    cp('dve', ident_bf[:], ident_f, [s_cf], [s_identbf])
    ts(nbf_f[:], bif[:, :, 1], -1.0, None, ALU.mult, None, [s_bif], [s_nbff])
    for kt in range(KT):
        b = next_prj_bank()
        tr(banks[b][:, 0:NR], rows_sb[0:NR, kt * 128:(kt + 1) * 128], ident_f[0:NR, 0:NR],
           [sI["rows"], s_cf], [s_bank[b]])
        cp('act', colsT[:, kt, :], banks[b][:, 0:NR], [s_bank[b]], [s_colsT])
    act(csT[:], colsT[:, :, r_c(0):r_c(0) + NSEQ], AF.Silu, [s_colsT], [s_csT])

    def prepass(src_tok_ap, ntok, dst_fn, key):
        P.dma('sp', xtok[0:ntok, :], src_tok_ap, "xtok", (), [sI["xtok"]])
        for g in range(4):
            b = next_prj_bank()
            for i in range(4):
                kt = g * 4 + i
                tr(banks[b][:, i * 128:i * 128 + ntok], xtok[0:ntok, kt * 128:(kt + 1) * 128],
                   ident_f[0:ntok, 0:ntok], [sI["xtok"], s_cf], [s_bank[b]])
            o = g % 2
            cp('act' if g % 2 == 0 else 'dve', xo4[o][:, :, 0:ntok],
               banks[b][:, :].rearrange("p (a b) -> p a b", a=4)[:, :, 0:ntok],
               [s_bank[b]], [sI["xo4_%d" % o]])
            P.dma('sp', dst_fn(g * 4), xo4[o][:, :, 0:ntok], "xo4_%d" % o, [sI["xo4_%d" % o]], ())

    for tb in range(T // 128):
        prepass(xp[tb * 128:(tb + 1) * 128, :], 128,
                lambda k0, tb=tb: xres_p[k0:k0 + 4, :, tb * 128:(tb + 1) * 128].rearrange("k p t -> p k t"), "pp")
    for s in range(NS):
        prepass(xs[s, :, :], SL,
                lambda k0, s=s: xres_s[s, k0:k0 + 4, :, :].rearrange("k p t -> p k t"), "ps")

    class Tile:
        pass

    tiles = []
    for j in range(T // MT):
        t = Tile()
        t.seq = 0; t.M = MT; t.Lc = 128; t.nch = MT // 128; t.t0 = j * MT
        t.first = (j == 0); t.last = (j == T // MT - 1); t.sample = None
        t.xres = lambda kt, t=t: xres_p[kt, :, t.t0:t.t0 + t.M]
        tiles.append(t)
    for s in range(NS):
        t = Tile()
        t.seq = 1 + s; t.M = SL; t.Lc = SL; t.nch = 1; t.t0 = 0
        t.first = True; t.last = True; t.sample = s
        t.xres = lambda kt, s=s: xres_s[s, kt, :, :]
        tiles.append(t)

    s_xres = {}

    def xres_slot(seq):
        if seq not in s_xres:
            s_xres[seq] = [Slot("xres%d_0" % seq), Slot("xres%d_1" % seq)]
        return s_xres[seq]

    pre_store_ops = [op for op in P.ops['sp'] if op.is_dma and op.dkey in ("xo4_0", "xo4_1")]
    last_pre = {}
    for op in pre_store_ops:
        last_pre[op.dkey] = op

    class _Multi:
        pass

    pre_slots = []
    for k, op in last_pre.items():
        sl = Slot("pre_" + k)
        sl.last_w = op
        pre_slots.append(sl)

    def rms_stats(tile, load_x, xbuf, xslots, sqbuf, sqslots):
        M = tile.M
        b = next_prj_bank()
        for kt in range(KT):
            i = kt % 2
            load_x(kt, xbuf[i], xslots[i])
            act(sqbuf[i][:, 0:M], xbuf[i][:, 0:M], AF.Square, [xslots[i]], [sqslots[i]])
            mm(banks[b][:, 0:M], ones_bf[:, :], sqbuf[i][:, 0:M], kt == 0, kt == KT - 1,
               [s_onesbf, sqslots[i]], [s_bank[b]])
        act(rstd_bc[:, 0:M], banks[b][:, 0:M], AF.Sqrt, [s_bank[b], s_epsc], [s_rstd],
            bias=epsc[:, 0:1], scale=1.0 / D)
        P.add('dve', lambda e: e.reciprocal(out=rstd_bc[:, 0:M], in_=rstd_bc[:, 0:M]), [s_rstd], [s_rstd])

    def layer(l):
        wcache_idx.clear()
        mb = 2
        for blk in range(3 * D // WCOLS):
            si, w = load_w(wada_d[l, :, blk * WCOLS:(blk + 1) * WCOLS], KT, WCOLS)
            for ct in range(WCOLS // 128):
                e = blk * (WCOLS // 128) + ct
                for kt in range(KT):
                    mm(banks[mb][:, e * NSEQ:(e + 1) * NSEQ], w[:, kt, ct * 128:(ct + 1) * 128], csT[:, kt, :],
                       kt == 0, kt == KT - 1, si + [s_csT], [s_bank[mb]])
        for wi in range(3):
            for q in range(NSEQ):
                src = banks[mb][:, wi * KT * NSEQ:(wi + 1) * KT * NSEQ].rearrange("p (k q) -> p k q", q=NSEQ)[:, :, q]
                tt(modT[:, wi * KT:(wi + 1) * KT, q], src, colsT[:, :, r_ada(l, wi)], ALU.add,
                   [s_bank[mb], s_colsT], [s_modT])
        for q in range(NSEQ):
            stt(AmodT[:, :, q], modT[:, KT:2 * KT, q], 1.0, colsT[:, :, r_norm(l)], ALU.add, ALU.mult,
                [s_modT, s_colsT], [s_AmodT])
        P.dma('pool', wpm[:], wpm_d[l].rearrange("g (k p) c -> p g k c", p=128), "wpm", (), [s_wpm])
        P.dma('pool', wg[:], win_d[l, :, OFF_IF:OFF_IF + 8].rearrange("(k p) c -> p k c", p=128), "wg", (), [s_wg])

        for tile in tiles:
            run_tile(l, tile)

    def run_tile(l, tile):
        M, Lc, nch, q = tile.M, tile.Lc, tile.nch, tile.seq
        xsl = xres_slot(q)

        def load_x(kt, buf, slot):
            P.dma('sp', buf[:, 0:M], tile.xres(kt), "ld_" + slot.name, xsl + pre_slots, [slot])

        rms_stats(tile, load_x, xa, [sA["xa0"], sA["xa1"]], sqb, [sA["sqb0"], sA["sqb1"]])
        for kt in range(KT):
            i = kt % 2
            load_x(kt, xa[i], sA["xa%d" % i])
            tt(ta[i][:, 0:M], xa[i][:, 0:M], rstd_bc[:, 0:M], ALU.mult, [sA["xa%d" % i], s_rstd], [sA["ta%d" % i]])
            act(hT[:, kt, 0:M], ta[i][:, 0:M], AF.Identity, [sA["ta%d" % i], s_AmodT, s_modT], [s_hT],
                bias=modT[:, kt, q:q + 1], scale=AmodT[:, kt, q:q + 1])

        stage_C(l, tile)
        stage_B(l, tile)
        stage_D(l, tile)

    def stage_C(l, tile):
        M, Lc, nch, q = tile.M, tile.Lc, tile.nch, tile.seq
        W = HIST + M
        if tile.first:
            if tile.sample is None:
                memset('dve', ucarry[:], 0.0, [s_ucarry])
            else:
                s = tile.sample
                P.dma('sp', hist_in[0:HIST, :], stP_d[l, s, :, :], "hist_in", (), [sC["zbT"]])
                for ct in range(8):
                    b = next_prj_bank()
                    tr(banks[b][:, 0:HIST], hist_in[0:HIST, ct * 128:(ct + 1) * 128], ident_f[0:HIST, 0:HIST],
                       [sC["zbT"], s_cf], [s_bank[b]])
                    cp('act', ucarry[:, ct, :], banks[b][:, 0:HIST], [s_bank[b]], [s_ucarry])
        for blk in range(DB // WCOLS):
            si, w = load_w(win_d[l, :, OFF_ZB + blk * WCOLS:OFF_ZB + (blk + 1) * WCOLS], KT, WCOLS, ('zb', blk))
            for ct in range(WCOLS // 128):
                c = blk * (WCOLS // 128) + ct
                b = next_prj_bank()
                for kt in range(KT):
                    mm(banks[b][:, 0:M], w[:, kt, ct * 128:(ct + 1) * 128], hT[:, kt, 0:M], kt == 0, kt == KT - 1,
                       si + [s_hT], [s_bank[b]])
                act(zbT[:, c, 0:M], banks[b][:, 0:M], AF.Silu, [s_bank[b]], [sC["zbT"]])
        for blk in range(DB // WCOLS):
            si, w = load_w(win_d[l, :, OFF_U + blk * WCOLS:OFF_U + (blk + 1) * WCOLS], KT, WCOLS, ('u', blk))
            for ct in range(WCOLS // 128):
                c = blk * (WCOLS // 128) + ct
                g = c // 2
                u = uT2[c % 2]
                us = sC["uT%d" % (c % 2)]
                b = next_prj_bank()
                for kt in range(KT):
                    mm(banks[b][:, 0:M], w[:, kt, ct * 128:(ct + 1) * 128], hT[:, kt, 0:M], kt == 0, kt == KT - 1,
                       si + [s_hT], [s_bank[b]])
                cp('dve', u[:, 0:HIST], ucarry[:, c, :], [s_ucarry], [us])
                cp('act', u[:, HIST:W], banks[b][:, 0:M], [s_bank[b]], [us])
                cp('dve', ucarry[:, c, :], u[:, M:W], [us], [s_ucarry])
                cur, curslot = u, us
                sh = 1
                for step in range(g + 1):
                    o = ptmp[step % 2]
                    oslot = sC["ptmp%d" % (step % 2)]
                    lo = 2 * sh - 1
                    tt(o[:, lo:W], cur[:, lo:W], cur[:, lo - sh:W - sh], ALU.add, [curslot], [oslot])
                    cur, curslot = o, oslot
                    sh *= 2
                wlen = 2 ** (g + 1)
                stt(dT[:, c, 0:M], cur[:, HIST:W], 1.0 / wlen, u[:, HIST:W], ALU.mult, ALU.subtract,
                    [curslot, us], [sC["dT"]])
                if tile.first and tile.sample is None:
                    n = min(HIST, M)
                    o2 = ptmp[(g + 1) % 2]
                    o2slot = sC["ptmp%d" % ((g + 1) % 2)]
                    tt(o2[:, 0:n], cur[:, HIST:HIST + n], invcnt[:, g * 16:g * 16 + n], ALU.mult, [curslot, s_cf],
                       [o2slot])
                    tt(dT[:, c, 0:n], o2[:, 0:n], u[:, HIST:HIST + n], ALU.subtract, [o2slot, us], [sC["dT"]])
        for c in range(8):
            g = c // 2
            b = next_prj_bank()
            for k2 in range(2):
                mm(banks[b][:, 0:M], wpm[:, g, k2, (c % 2) * 128:(c % 2) * 128 + 128], dT[:, 2 * g + k2, 0:M],
                   k2 == 0, k2 == 1, [s_wpm, sC["dT"]], [s_bank[b]])
            stt(bbT[:, c, 0:M], banks[b][:, 0:M], colsT[:, c, r_ps(l):r_ps(l) + 1], zbT[:, c, 0:M], ALU.mult, ALU.mult,
                [s_bank[b], s_colsT, sC["zbT"]], [s_bbT])
        if tile.last:
            for ct in range(8):
                b = next_prj_bank()
                tr(banks[b][0:HIST, 0:128], ucarry[:, ct, :], ident_f[:, :], [s_ucarry, s_cf], [s_bank[b]])
                cp('act', hist_out[0:HIST, ct * 128:(ct + 1) * 128], banks[b][0:HIST, 0:128], [s_bank[b]],
                   [sC["dT"]])
            dst = pp_d[l, :, :] if tile.sample is None else ps_d[l, tile.sample, :, :]
            P.dma('sp', dst, hist_out[0:HIST, :], "hist_out", [sC["dT"]], ())

    ucarry = sb("ucarry", [128, 8, HIST], F32)
    s_ucarry = Slot("ucarry")

    def stage_B(l, tile):
        M, Lc, nch, q = tile.M, tile.Lc, tile.nch, tile.seq
        bi, bf_ = 2, 3
        for kt in range(KT):
            mm(banks[bi][0:4, 0:M], wg[:, kt, 0:4], hT[:, kt, 0:M], kt == 0, kt == KT - 1, [s_wg, s_hT], [s_bank[bi]])
        for kt in range(KT):
            mm(banks[bf_][0:4, 0:M], wg[:, kt, 4:8], hT[:, kt, 0:M], kt == 0, kt == KT - 1, [s_wg, s_hT], [s_bank[bf_]])
        act(g_ig[:, 0:M], banks[bi][0:4, 0:M], AF.Identity, [s_bank[bi], s_bif], [s_gig], bias=bif[:, l, 0:1])
        act(g_lf[:, 0:M], banks[bf_][0:4, 0:M], AF.Exp, [s_bank[bf_], s_nbff], [s_glf], bias=nbf_f[:, l:l + 1], scale=-1.0)
        act(g_lf[:, 0:M], g_lf[:, 0:M], AF.Ln, [s_glf, s_ones4], [s_glf], bias=ones4[:, 0:1])
        ts(g_lf[:, 0:M], g_lf[:, 0:M], -1.0, None, ALU.mult, None, [s_glf], [s_glf])
        if tile.first:
            if tile.sample is None:
                memset('dve', mcar[:], 0.0, [s_mcar])
            else:
                cp('dve', mcar[:], mst[:, l * NS + tile.sample:l * NS + tile.sample + 1], [s_mst], [s_mcar])
        ts(cend[:, 0:1], mcar[:], -1.0, None, ALU.mult, None, [s_mcar], [s_cend])
        P.add('dve', lambda e: e.tensor_tensor_scan(out=g_B[:, 0:M], data0=g_lf[:, 0:M], data1=g_lf[:, 0:M],
                                                    initial=0.0, op0=ALU.add, op1=ALU.add),
              [s_glf], [s_gB])
        P.add('dve', lambda e: e.tensor_tensor_scan(out=g_m[:, 0:M], data0=g_lf[:, 0:M], data1=g_ig[:, 0:M],
                                                    initial=mcar[:, 0:1], op0=ALU.add, op1=ALU.max),
              [s_glf, s_gig, s_mcar], [s_gm])
        stt(g_ig[:, 0:M], g_B[:, 0:M], -0.5, g_ig[:, 0:M], ALU.mult, ALU.add, [s_gig, s_gB], [s_gig])
        stt(g_B[:, 0:M], g_B[:, 0:M], 0.5, g_m[:, 0:M], ALU.mult, ALU.subtract, [s_gB, s_gm], [s_gB])
        cp('dve', mcar[:], g_m[:, M - 1:M], [s_gm], [s_mcar])
        cp('dve', cend[:, 1:nch + 1], g_c[:, Lc - 1:M:Lc], [s_gc], [s_cend])
        for h in range(NH):
            ts(cediag[:, h, 0:nch + 1], cend[:, 0:nch + 1], ident_f[0:4, h:h + 1], None, ALU.mult, None,
               [s_cend, s_cf], [s_cediag])
        cb = 4
        mm(banks[cb][:, 0:NH * (NCH + 1)], ones4[:, :], cediag[:].rearrange("p h j -> p (h j)"), True, True,
           [s_ones4, s_cediag], [s_bank[cb]])
        cp('act', CE[:].rearrange("p h j -> p (h j)"), banks[cb][:, 0:NH * (NCH + 1)], [s_bank[cb]], [s_CE])
        tb = 5
        for j in range(nch):
            for wi, (rowt, rslot) in enumerate([(g_g, s_gg), (g_c, s_gc), (g_m, s_gm)]):
                tr(banks[tb][0:Lc, (j * 3 + wi) * 4:(j * 3 + wi) * 4 + 4], rowt[0:4, j * Lc:(j + 1) * Lc],
                   ident_f[0:4, 0:4], [rslot, s_cf], [s_bank[tb]])
        cp('act', cols[0:Lc, 0:nch, :, :].rearrange("p a b c -> p (a b c)"), banks[tb][0:Lc, 0:nch * 12],
           [s_bank[tb]], [s_cols])
        CEp = CE[:, :, 0:nch].rearrange("p h j -> p j h")
        CEc = CE[:, :, 1:nch + 1].rearrange("p h j -> p j h")
        tt(tmpc[0:Lc, 0:nch, :], cols[0:Lc, 0:nch, 1, :], CEp[0:Lc], ALU.subtract, [s_cols, s_CE], [s_tmpc])
        act(acol[0:Lc, 0:nch, :], tmpc[0:Lc, 0:nch, :], AF.Exp, [s_tmpc], [s_acol])
        act(emcol[0:Lc, 0:nch, :], cols[0:Lc, 0:nch, 2, :], AF.Exp, [s_cols], [s_emcol], scale=-1.0)
        tt(tmpc[0:Lc, 0:nch, :], cols[0:Lc, 0:nch, 0, :], CEc[0:Lc], ALU.add, [s_cols, s_CE], [s_tmpc])
        act(wecol[0:Lc, 0:nch, :], tmpc[0:Lc, 0:nch, :], AF.Exp, [s_tmpc], [s_wecol])
        tt(tmpc[:, 0:nch, :], CEc, CEp, ALU.subtract, [s_CE], [s_tmpc])
        act(dccol[:, 0:nch, :], tmpc[:, 0:nch, :], AF.Exp, [s_tmpc], [s_dccol])

        if tile.first:
            if tile.sample is None:
                for h in range(NH):
                    for dt in range(4):
                        memset('dve', Cst[:, h, dt, :], 0.0, [s_Cst[h][dt]])
                memset('dve', nst[:], 0.0, [s_nst])
            else:
                s = tile.sample
                for h in range(NH):
                    P.dma('sp', Cst[:, h, :, :], stC_d[l, s, h].rearrange("(k p) e -> p k e", p=128), "Cld%d" % h,
                          (), s_Cst[h])
                cp('dve', nst[:].rearrange("p h d -> p (h d)"), colsT[:, :, r_n(l, s)], [s_colsT], [s_nst])

        for h in range(NH):
            head(l, tile, h)

        if tile.last:
            s = tile.sample
            for h in range(NH):
                dst = Cp_d[l, h] if s is None else Cs_d[l, s, h]
                P.dma('sp', dst.rearrange("(k p) e -> p k e", p=128), Cst[:, h, :, :], "Cst_out%d" % h, s_Cst[h], ())
            nb = 4
            tr(banks[nb][0:16, 0:128], nst[:].rearrange("p h d -> p (h d)"), ident_f[:, :], [s_nst, s_cf], [s_bank[nb]])
            sm = small[0:16, 1, :]
            cp('act', nout[0:16, :], banks[nb][0:16, 0:128], [s_bank[nb]], [s_nout])
            dstn = np_d[l] if s is None else ns_d[l, s]
            P.dma('sp', dstn, nout[0:16, :], "nout", [s_nout], ())
            dstm = mp_d[l:l + 1, :] if s is None else ms_d[l, s:s + 1, :]
            P.dma('sp', dstm.rearrange("o k -> k o"), mcar[:, 0:1], "mout", [s_mcar], ())

    nout = sb("nout", [16, 128], F32)
    s_nout = Slot("nout")

    def proj_fm(l, tile, off, dstT, dslot, evac):
        M = tile.M
        si, w = load_w(win_d[l, :, off:off + WCOLS], KT, WCOLS, ('fm', off))
        for ct in range(4):
            b = next_prj_bank()
            for kt in range(KT):
                mm(banks[b][:, 0:M], w[:, kt, ct * 128:(ct + 1) * 128], hT[:, kt, 0:M], kt == 0, kt == KT - 1,
                   si + [s_hT], [s_bank[b]])
            evac(ct, b)

    def proj_tm(l, tile, off, evac):
        M, Lc, nch = tile.M, tile.Lc, tile.nch
        si, w = load_w(win_d[l, :, off:off + WCOLS], KT, WCOLS, ('tm', off))
        for j in range(nch):
            b = next_prj_bank()
            for kt in range(KT):
                mm(banks[b][0:Lc, 0:DH], hT[:, kt, j * Lc:(j + 1) * Lc], w[:, kt, :], kt == 0, kt == KT - 1,
                   si + [s_hT], [s_bank[b]])
            evac(j, b)

    def head(l, tile, h):
        M, Lc, nch, q = tile.M, tile.Lc, tile.nch, tile.seq
        proj_fm(l, tile, OFF_Q + h * DH, qT, sB["qT"],
                lambda ct, b: cp('act', qT[:, ct, 0:M], banks[b][:, 0:M], [s_bank[b]], [sB["qT"]]))
        proj_fm(l, tile, OFF_K + h * DH, kT, sB["kT"],
                lambda ct, b: act(kT[:, ct, 0:M], banks[b][:, 0:M], AF.Identity, [s_bank[b]], [sB["kT"]],
                                  scale=float(DH) ** -0.5))
        proj_tm(l, tile, OFF_V + h * DH,
                lambda j, b: cp('dve', vtk[0:Lc, j, :], banks[b][0:Lc, 0:DH], [s_bank[b]], [sB["v"]]))
        proj_tm(l, tile, OFF_O + h * DH,
                lambda j, b: act(sgo[0:Lc, j, :], banks[b][0:Lc, 0:DH], AF.Sigmoid, [s_bank[b]], [sB["sgo"]]))
        proj_tm(l, tile, OFF_ZA + h * DH,
                lambda j, b: act(sza[0:Lc, j, :], banks[b][0:Lc, 0:DH], AF.Silu, [s_bank[b]], [sB["sza"]]))
        for dt in range(4):
            cp('act', Cbf[:, dt, :], Cst[:, h, dt, :], [s_Cst[h][dt]], [s_Cbf[dt]])
        cp('dve', nbf[:, h, :], nst[:, h, :], [s_nst], [s_nbf])

        deferred = []

        def emit_a_transposes(jj):
            ii = jj % 2
            cj = slice(jj * Lc, (jj + 1) * Lc)
            bt = next_prj_bank()
            btv = banks[bt][:, :].bitcast(BF16)
            for i in range(4):
                tr(btv[:, i * 128:i * 128 + Lc], atok[ii][0:Lc, i * 128:(i + 1) * 128], ident_bf[0:Lc, 0:Lc],
                   [sB2["atok"][ii], s_identbf], [s_bank[bt]])
            for i in range(4):
                act(aT[:, h * 4 + i, cj], btv[:, i * 128:i * 128 + Lc], AF.Identity, [s_bank[bt], s_colsT], [s_aT],
                    scale=colsT[:, h * 4 + i, r_hn(l):r_hn(l) + 1])

        for j in range(nch):
            i2 = j % 2
            cs = slice(j * Lc, (j + 1) * Lc)
            bS, bC = 2, 3
            bA, bB = (4, 5) if j % 2 == 0 else (6, 7)
            for dt in range(4):
                mm(banks[bS][0:Lc, 0:Lc], kT[:, dt, cs], qT[:, dt, cs], dt == 0, dt == 3,
                   [sB["kT"], sB["qT"]], [s_bank[bS]])
            mm(banks[bC][0:Lc, 0:Lc], sel[0:4, h * 128:h * 128 + Lc], g_c[0:4, cs], True, False,
               [s_sel, s_gc], [s_bank[bC]])
            mm(banks[bC][0:Lc, 0:Lc], ident_f[0:Lc, 0:Lc], maskneg[0:Lc, 0:Lc], False, True,
               [s_cf], [s_bank[bC]])
            for dt in range(4):
                mm(banks[bA][0:Lc, 0:DH], qT[:, dt, cs], Cbf[:, dt, :], dt == 0, dt == 3,
                   [sB["qT"], s_Cbf[dt]], [s_bank[bA]])
            act(DTm[i2][0:Lc, 0:Lc], banks[bC][0:Lc, 0:Lc], AF.Exp, [s_bank[bC], s_cols], [sB2["DT"][i2]],
                bias=cols[0:Lc, j, 0, h:h + 1])
            tt(PTm[i2][0:Lc, 0:Lc], banks[bS][0:Lc, 0:Lc], DTm[i2][0:Lc, 0:Lc], ALU.mult,
               [s_bank[bS], sB2["DT"][i2]], [sB2["PT"][i2]])
            mm(banks[bB][0:Lc, 0:DH], PTm[i2][0:Lc, 0:Lc], vtk[0:Lc, j, :], True, True,
               [sB2["PT"][i2], sB["v"]], [s_bank[bB]])
            for dt in range(4):
                mm(banks[bS][0:Lc, 128:129], qT[:, dt, cs], nbf[:, h, dt:dt + 1], dt == 0, dt == 3,
                   [sB["qT"], s_nbf], [s_bank[bS]])
            mm(banks[bS][0:Lc, 129:130], PTm[i2][0:Lc, 0:Lc], ones_bf[0:Lc, 0:1], True, True,
               [sB2["PT"][i2], s_onesbf], [s_bank[bS]])
            sm = small[:, i2, :]
            ssl = s_small[i2]
            cp('act', sm[0:Lc, 0:2], banks[bS][0:Lc, 128:130], [s_bank[bS]], [ssl])
            bk = next_prj_bank()
            bkv = banks[bk][:, :].bitcast(BF16)
            for dt in range(4):
                tr(bkv[0:Lc, dt * 128:(dt + 1) * 128], kT[:, dt, cs], ident_bf[:, :], [sB["kT"], s_identbf],
                   [s_bank[bk]])
            act(kw[i2][0:Lc, :], bkv[0:Lc, 0:DH], AF.Identity, [s_bank[bk], s_wecol], [sB2["kw"][i2]],
                scale=wecol[0:Lc, j, h:h + 1])
            for dt in range(4):
                bu = next_prj_bank()
                mm(banks[bu][:, 0:DH], kw[i2][0:Lc, dt * 128:(dt + 1) * 128], vtk[0:Lc, j, :], True, True,
                   [sB2["kw"][i2], sB["v"]], [s_bank[bu]])
                stt(Cst[:, h, dt, :], Cst[:, h, dt, :], dccol[:, j, h:h + 1], banks[bu][:, 0:DH], ALU.mult, ALU.add,
                    [s_Cst[h][dt], s_dccol, s_bank[bu]], [s_Cst[h][dt]])
                if j < nch - 1:
                    cp('act', Cbf[:, dt, :], Cst[:, h, dt, :], [s_Cst[h][dt]], [s_Cbf[dt]])
            for dt in range(4):
                mm(banks[bC][:, 128 + dt:129 + dt], kw[i2][0:Lc, dt * 128:(dt + 1) * 128], ones_bf[0:Lc, 0:1],
                   True, True, [sB2["kw"][i2], s_onesbf], [s_bank[bC]])
            stt(nst[:, h, :], nst[:, h, :], dccol[:, j, h:h + 1], banks[bC][:, 128:132], ALU.mult, ALU.add,
                [s_nst, s_dccol, s_bank[bC]], [s_nst])
            if j < nch - 1:
                cp('dve', nbf[:, h, :], nst[:, h, :], [s_nst], [s_nbf])
            for jj in deferred:
                emit_a_transposes(jj)
            deferred = []
            stt(sm[0:Lc, 2:3], sm[0:Lc, 0:1], acol[0:Lc, j, h:h + 1], sm[0:Lc, 1:2], ALU.mult, ALU.add,
                [ssl, s_acol], [ssl])
            act(sm[0:Lc, 6:7], sm[0:Lc, 2:3], AF.Abs, [ssl], [ssl])
            tt(sm[0:Lc, 3:4], sm[0:Lc, 6:7], emcol[0:Lc, j, h:h + 1], ALU.max, [ssl, s_emcol], [ssl])
            P.add('dve', lambda e, sm=sm: e.reciprocal(out=sm[0:Lc, 4:5], in_=sm[0:Lc, 3:4]), [ssl], [ssl])
            tt(sm[0:Lc, 5:6], sm[0:Lc, 4:5], acol[0:Lc, j, h:h + 1], ALU.mult, [ssl, s_acol], [ssl])
            act(A2[i2][0:Lc, :], banks[bA][0:Lc, 0:DH], AF.Identity, [s_bank[bA], ssl], [sB2["A2"][i2]],
                scale=sm[0:Lc, 5:6])
            stt(hm[i2][0:Lc, :], banks[bB][0:Lc, 0:DH], sm[0:Lc, 4:5], A2[i2][0:Lc, :], ALU.mult, ALU.add,
                [s_bank[bB], ssl, sB2["A2"][i2]], [sB2["hm"][i2]])
            tt(hm[i2][0:Lc, :], hm[i2][0:Lc, :], sgo[0:Lc, j, :], ALU.mult, [sB2["hm"][i2], sB["sgo"]], [sB2["hm"][i2]])
            bs_ = bnst[:, i2, :]
            bsl = s_bnst[i2]
            P.add('dve', lambda e, bs_=bs_, x=hm[i2]: e.bn_stats(out=bs_[0:Lc, 0:6], in_=x[0:Lc, :]),
                  [sB2["hm"][i2]], [bsl])
            P.add('dve', lambda e, bs_=bs_: e.bn_aggr(out=bs_[0:Lc, 6:8], in_=bs_[0:Lc, 0:6]), [bsl], [bsl])
            act(sm[0:Lc, 8:9], bs_[0:Lc, 7:8], AF.Sqrt, [bsl, s_epsc], [ssl], bias=epsc[0:Lc, 0:1])
            P.add('dve', lambda e, sm=sm: e.reciprocal(out=sm[0:Lc, 9:10], in_=sm[0:Lc, 8:9]), [ssl], [ssl])
            ts(A2[i2][0:Lc, :], hm[i2][0:Lc, :], bs_[0:Lc, 6:7], sm[0:Lc, 9:10], ALU.subtract, ALU.mult,
               [sB2["hm"][i2], bsl, ssl], [sB2["A2"][i2]])
            tt(atok[i2][0:Lc, :], A2[i2][0:Lc, :], sza[0:Lc, j, :], ALU.mult, [sB2["A2"][i2], sB["sza"]],
               [sB2["atok"][i2]])
            deferred.append(j)
        for jj in deferred:
            emit_a_transposes(jj)

    def stage_D(l, tile):
        M, Lc, nch, q = tile.M, tile.Lc, tile.nch, tile.seq
        xsl = xres_slot(q)
        HC = WCOLS // 2
        for jb in range(D // HC):
            c0 = jb * HC
            sa, wga = load_w(win_d[l, :, OFF_GA + c0:OFF_GA + c0 + HC], KT, HC, ('ga', jb))
            sb_, wgb = load_w(win_d[l, :, OFF_GB + c0:OFF_GB + c0 + HC], KT, HC, ('gb', jb))
            sA_, wA = load_w(wa_d[l, :, c0:c0 + HC], KT, HC, ('wa', jb))
            sB_, wB = load_w(wb_d[l, :, c0:c0 + HC], 8, HC, ('wb', jb))
            for ct in range(2):
                jt = jb * 2 + ct
                csl = slice(ct * 128, (ct + 1) * 128)
                i2 = jt % 2
                b1, b2, b3, b4 = 2, 3, 4, 5
                for kt in range(KT):
                    mm(banks[b1][:, 0:M], wga[:, kt, csl], hT[:, kt, 0:M], kt == 0, kt == KT - 1,
                       sa + [s_hT], [s_bank[b1]])
                act(sg[i2][:, 0:M], banks[b1][:, 0:M], AF.Sigmoid, [s_bank[b1]], [sD["sg%d" % i2]])
                for kt in range(KT):
                    mm(banks[b2][:, 0:M], wgb[:, kt, csl], hT[:, kt, 0:M], kt == 0, kt == KT - 1,
                       sb_ + [s_hT], [s_bank[b2]])
                act(sg[2 + i2][:, 0:M], banks[b2][:, 0:M], AF.Sigmoid, [s_bank[b2]], [sD["sg%d" % (2 + i2)]])
                for kt in range(KT):
                    mm(banks[b3][:, 0:M], wA[:, kt, csl], aT[:, kt, 0:M], kt == 0, kt == KT - 1,
                       sA_ + [s_aT], [s_bank[b3]])
                tt(t12[0][:, 0:M], banks[b3][:, 0:M], sg[i2][:, 0:M], ALU.mult, [s_bank[b3], sD["sg%d" % i2]],
                   [sD["t12_0"]])
                for kt in range(8):
                    mm(banks[b4][:, 0:M], wB[:, kt, csl], bbT[:, kt, 0:M], kt == 0, kt == 7,
                       sB_ + [s_bbT], [s_bank[b4]])
                tt(t12[1][:, 0:M], banks[b4][:, 0:M], sg[2 + i2][:, 0:M], ALU.mult,
                   [s_bank[b4], sD["sg%d" % (2 + i2)]], [sD["t12_1"]])
                tt(mgT[:, jt, 0:M], t12[0][:, 0:M], t12[1][:, 0:M], ALU.add, [sD["t12_0"], sD["t12_1"]], [sD["mgT"]])
        for jb in range(D // WCOLS):
            c0 = jb * WCOLS
            so, wo = load_w(wo_d[l, :, c0:c0 + WCOLS], KT, WCOLS, ('wo', jb))
            for ct in range(4):
                jt = jb * 4 + ct
                csl = slice(ct * 128, (ct + 1) * 128)
                i2 = jt % 2
                b = next_prj_bank()
                P.dma('sp', xin[i2][:, 0:M], tile.xres(jt), "xin%d" % i2, xsl + pre_slots, [sD["xin%d" % i2]])
                for kt in range(KT):
                    mm(banks[b][:, 0:M], wo[:, kt, csl], mgT[:, kt, 0:M], kt == 0, kt == KT - 1,
                       so + [sD["mgT"]], [s_bank[b]])
                stt(xout[i2][:, 0:M], banks[b][:, 0:M], modT[:, 2 * KT + jt, q:q + 1], xin[i2][:, 0:M], ALU.mult, ALU.add,
                    [s_bank[b], s_modT, sD["xin%d" % i2]], [sD["xout%d" % i2]])
                P.dma('sp', tile.xres(jt), xout[i2][:, 0:M], "xout%d" % i2, [sD["xout%d" % i2]], [xsl[i2]])

    def final_tile(tile):
        M, Lc, nch, q = tile.M, tile.Lc, tile.nch, tile.seq
        xsl = xres_slot(q)

        def load_x(kt, buf, slot):
            P.dma('sp', buf[:, 0:M], tile.xres(kt), "ld_" + slot.name, xsl + pre_slots, [slot])

        rms_stats(tile, load_x, fxa, [sF["fxa0"], sF["fxa1"]], fsq, [sF["fsq0"], sF["fsq1"]])
        for j in range(nch):
            yo = j % 2
            cs = slice(j * Lc, (j + 1) * Lc)
            for g in range(4):
                b = next_prj_bank()
                for i in range(4):
                    kt = g * 4 + i
                    i2 = kt % 2
                    P.dma('sp', fxa[i2][:, 0:Lc], tile.xres(kt)[:, cs], "ld_" + sF["fxa%d" % i2].name,
                          xsl + pre_slots, [sF["fxa%d" % i2]])
                    stt(fta[i2][:, 0:Lc], fxa[i2][:, 0:Lc], colsT[:, kt, r_fin:r_fin + 1], rstd_bc[:, cs],
                        ALU.mult, ALU.mult, [sF["fxa%d" % i2], s_colsT, s_rstd], [sF["fta%d" % i2]])
                    tr(banks[b][0:Lc, i * 128:(i + 1) * 128], fta[i2][:, 0:Lc], ident_f[:, :],
                       [sF["fta%d" % i2], s_cf], [s_bank[b]])
                cp('act' if g % 2 == 0 else 'dve', ytok[yo][0:Lc, g * 512:(g + 1) * 512], banks[b][0:Lc, :],
                   [s_bank[b]], [sF["ytok%d" % yo]])
            if tile.sample is None:
                dst = yp_d[tile.t0 + j * Lc:tile.t0 + (j + 1) * Lc, :]
            else:
                dst = ys_d[tile.sample, :, :]
            P.dma('sp', dst, ytok[yo][0:Lc, :], "ytok%d" % yo, [sF["ytok%d" % yo]], ())

    for l in range(L):
        layer(l)
    for tile in tiles:
        final_tile(tile)

    P.emit()
    return nc


_CACHE = {}


def _consts():
    cf = np.zeros((128, 320), np.float32)
    cf[:, 0:128] = np.eye(128, dtype=np.float32)
    s = np.arange(128)[:, None]
    t = np.arange(128)[None, :]
    cf[:, 128:256] = np.where(s <= t, 0.0, -30000.0).astype(np.float32)
    for g, w in enumerate((2, 4, 8, 16)):
        for tt_ in range(16):
            cf[:, 256 + g * 16 + tt_] = 1.0 / min(w, tt_ + 1)
    sel = np.zeros((4, 4, 128), np.float32)
    for h in range(4):
        sel[h, h, :] = 1.0
    return cf, sel.reshape(4, 512)


def kernel(x_prompt, x_sample, c_prompt, c_sample, state_C, state_n, state_m, state_pool,
           norm_w, w_ada, b_ada, w_in, b_if, head_norm_w, w_pool_mix, pool_scale,
           w_branch_a, w_branch_b, w_out, final_norm_w):
    f = lambda a: np.ascontiguousarray(np.asarray(a, dtype=np.float32))
    x_prompt, x_sample, c_prompt, c_sample = f(x_prompt), f(x_sample), f(c_prompt), f(c_sample)
    state_C, state_n, state_m, state_pool = f(state_C), f(state_n), f(state_m), f(state_pool)
    norm_w, w_ada, b_ada, w_in, b_if = f(norm_w), f(w_ada), f(b_ada), f(w_in), f(b_if)
    head_norm_w, w_pool_mix, pool_scale = f(head_norm_w), f(w_pool_mix), f(pool_scale)
    w_branch_a, w_branch_b, w_out, final_norm_w = f(w_branch_a), f(w_branch_b), f(w_out), f(final_norm_w)

    B, T, _ = x_prompt.shape
    SB, SL, _ = x_sample.shape
    L = w_in.shape[0]
    NCORES = 8
    NS = SB // B
    assert B * 2 == NCORES and SB == B * NS
    key = (T, NS, SL, L)
    if key not in _CACHE:
        _CACHE[key] = build_program(T, NS, SL, L)
    nc = _CACHE[key]
    NSEQ = 1 + NS
    NR = 6 * L + 1 + NSEQ + L * NS
    cf, sel = _consts()
    real = [0, 2, 4, 6]
    zcache = {}

    def zeros_like(a):
        k = (a.shape, a.dtype.str)
        if k not in zcache:
            zcache[k] = np.zeros(a.shape, a.dtype)
        return zcache[k]

    in_maps = []
    for core in range(NCORES):
        c = core // 2
        rows = np.zeros((NR, D), np.float32)
        for l in range(L):
            rows[l] = norm_w[l]
            rows[L + 3 * l:L + 3 * l + 3] = b_ada[l].reshape(3, D)
            rows[4 * L + l] = head_norm_w[l]
            rows[5 * L + l, 0:DB] = pool_scale[l]
        rows[6 * L] = final_norm_w
        rows[6 * L + 1] = c_prompt[c]
        for s in range(NS):
            rows[6 * L + 2 + s] = c_sample[c * NS + s]
        for l in range(L):
            for s in range(NS):
                rows[6 * L + 1 + NSEQ + l * NS + s] = state_n[l, c * NS + s].reshape(-1)
        m = {
            "xp": x_prompt[c], "xs": x_sample[c * NS:(c + 1) * NS], "rows": rows, "bif": b_if,
            "st_m": state_m[:, c * NS:(c + 1) * NS], "st_C": state_C[:, c * NS:(c + 1) * NS],
            "st_pool": state_pool[:, c * NS:(c + 1) * NS], "w_ada": w_ada, "w_in": w_in, "w_pm": w_pool_mix,
            "w_a": w_branch_a, "w_b": w_branch_b, "w_o": w_out, "cf": cf, "sel": sel,
        }
        m = {k_: np.ascontiguousarray(v_) for k_, v_ in m.items()}
        if core not in real:
            m = {k_: zeros_like(v_) for k_, v_ in m.items()}
        in_maps.append(m)
    res = run_bass_kernel_spmd(nc, in_maps, core_ids=list(range(NCORES)))
    R = [res.results[i] for i in real]
    y_prompt = np.stack([R[c]["yp"] for c in range(B)])
    y_sample = np.concatenate([R[c]["ys"] for c in range(B)], axis=0)
    Cp = np.stack([R[c]["Cp"] for c in range(B)], axis=1)
    np_ = np.stack([R[c]["np"].reshape(L, NH, DH) for c in range(B)], axis=1)
    mp = np.stack([R[c]["mp"] for c in range(B)], axis=1)
    pp = np.stack([R[c]["pp"] for c in range(B)], axis=1)
    Cs = np.concatenate([R[c]["Cs"] for c in range(B)], axis=1)
    ns = np.concatenate([R[c]["ns"].reshape(L, NS, NH, DH) for c in range(B)], axis=1)
    ms = np.concatenate([R[c]["ms"] for c in range(B)], axis=1)
    ps = np.concatenate([R[c]["ps"] for c in range(B)], axis=1)
    return (y_prompt.astype(np.float32), y_sample.astype(np.float32), Cp.astype(np.float32),
            np_.astype(np.float32), mp.astype(np.float32), pp.astype(np.float32), Cs.astype(np.float32),
            ns.astype(np.float32), ms.astype(np.float32), ps.astype(np.float32))
```
